# Optimizing a Trainium2 kernel written in Bass

```python
import jax, jax.numpy as jnp
from jax import lax
import numpy as np

D_MODEL = 1024
BATCH = 32
SEQ = 2048
DEPTH = 4

GRID_W = 64
CTX_LEN = 256
HEAD_DIM = 64
ROPE_THETA = 10000.0
A_HEADS = 6
A_KV_HEADS = 2
B_CH = 256
CONV_W = 31
C_HEADS = 6
C_KV_HEADS = 2
WINDOW = 128
BLOCK = 128
A_Q = A_HEADS * HEAD_DIM
A_KV = A_KV_HEADS * HEAD_DIM
C_Q = C_HEADS * HEAD_DIM
C_KV = C_KV_HEADS * HEAD_DIM
D_MIX = A_Q + B_CH + C_Q
D_IN = A_Q + 2 * A_KV + 2 * B_CH + C_Q + 2 * C_KV
N_GROUPS = 4
EXPERTS_PER_GROUP = 8
N_EXPERTS = N_GROUPS * EXPERTS_PER_GROUP
TOP_K = 2
D_EXPERT = 512
MOE_CHUNK = 256
N_MOD = 6
EPS = 1e-6
ATTN_SCALE = HEAD_DIM ** -0.5

kernel_name = "hymba_diffusion_hybrid_hmoe"


def rms_norm(x, g):
    xf = x.astype(jnp.float32)
    y = xf * lax.rsqrt(jnp.mean(xf * xf, axis=-1, keepdims=True) + EPS)
    return (y * g.astype(jnp.float32)).astype(x.dtype)


def layer_norm(x, g, b):
    xf = x.astype(jnp.float32)
    mu = jnp.mean(xf, axis=-1, keepdims=True)
    xc = xf - mu
    var = jnp.mean(xc * xc, axis=-1, keepdims=True)
    return (xc * lax.rsqrt(var + EPS) * g.astype(jnp.float32) + b.astype(jnp.float32)).astype(x.dtype)


def modulate(h, shift, scale):
    return h * (1 + scale) + shift


def heads(t):
    return t.reshape(*t.shape[:-1], -1, HEAD_DIM)


def split_proj(p):
    sizes = [A_Q, A_KV, A_KV, 2 * B_CH, C_Q, C_KV, C_KV]
    idx = [int(i) for i in np.cumsum(sizes)[:-1]]
    return jnp.split(p, idx, axis=-1)


def grid_rope(n_tokens):
    rows = n_tokens // GRID_W
    pos_row = jnp.repeat(jnp.arange(rows, dtype=jnp.float32), GRID_W)
    pos_col = jnp.tile(jnp.arange(GRID_W, dtype=jnp.float32), rows)
    n_freq = HEAD_DIM // 4
    inv = ROPE_THETA ** (-jnp.arange(n_freq, dtype=jnp.float32) / n_freq)
    ang = jnp.concatenate([pos_row[:, None] * inv, pos_col[:, None] * inv], axis=-1)
    return jnp.cos(ang), jnp.sin(ang)


def apply_rope(x, cos, sin):
    b, s, h, d = x.shape
    xr = x.reshape(b, s, h, 2, 2, d // 4)
    x1, x2 = xr[..., 0, :], xr[..., 1, :]
    c = cos.reshape(s, 1, 2, d // 4).astype(x.dtype)
    sn = sin.reshape(s, 1, 2, d // 4).astype(x.dtype)
    return jnp.stack([x1 * c - x2 * sn, x2 * c + x1 * sn], axis=-2).reshape(b, s, h, d)


def global_attention(q, k, v, k_ctx, v_ctx):
    b, s, h, d = q.shape
    kv = k.shape[2]
    g = h // kv
    nb = s // BLOCK
    k_all = jnp.concatenate([k, k_ctx], axis=1)
    v_all = jnp.concatenate([v, v_ctx], axis=1)
    qb = q.reshape(b, nb, BLOCK, kv, g, d).transpose(1, 0, 2, 3, 4, 5)

    def one_block(qblk):
        sc = jnp.einsum('bqhgd,bkhd->bhgqk', qblk, k_all).astype(jnp.float32) * ATTN_SCALE
        p = jax.nn.softmax(sc, axis=-1).astype(v_all.dtype)
        return jnp.einsum('bhgqk,bkhd->bqhgd', p, v_all)

    o = lax.map(one_block, qb)
    return o.transpose(1, 0, 2, 3, 4, 5).reshape(b, s, h * d)


def window_attention(q, k, v, k_ctx, v_ctx, sink):
    b, s, h, d = q.shape
    kv = k.shape[2]
    g = h // kv
    nb = s // BLOCK
    lk = k_ctx.shape[1]
    nband = 3 * BLOCK

    def bands(t):
        tp = jnp.pad(t, ((0, 0), (WINDOW, WINDOW), (0, 0), (0, 0)))
        tb = tp.reshape(b, nb + 2, BLOCK, kv, d).transpose(1, 0, 2, 3, 4)
        return jnp.concatenate([tb[:-2], tb[1:-1], tb[2:]], axis=2)

    qpos = jnp.arange(s).reshape(nb, BLOCK)
    kpos = jnp.arange(-WINDOW, s + WINDOW).reshape(nb + 2, BLOCK)
    kpos = jnp.concatenate([kpos[:-2], kpos[1:-1], kpos[2:]], axis=1)
    rel = kpos[:, None, :] - qpos[:, :, None]
    mask = (jnp.abs(rel) <= WINDOW) & (kpos[:, None, :] >= 0) & (kpos[:, None, :] < s)
    qb = q.reshape(b, nb, BLOCK, kv, g, d).transpose(1, 0, 2, 3, 4, 5)
    sink_l = sink.astype(jnp.float32).reshape(kv, g, 1, 1)

    def one_block(args):
        qblk, kblk, vblk, mblk = args
        sl = jnp.einsum('bqhgd,bkhd->bhgqk', qblk, kblk).astype(jnp.float32) * ATTN_SCALE
        sl = jnp.where(mblk, sl, -jnp.inf)
        sc = jnp.einsum('bqhgd,bkhd->bhgqk', qblk, k_ctx).astype(jnp.float32) * ATTN_SCALE
        sk = jnp.broadcast_to(sink_l, sl.shape[:-1] + (1,))
        p = jax.nn.softmax(jnp.concatenate([sl, sc, sk], axis=-1), axis=-1).astype(vblk.dtype)
        return (jnp.einsum('bhgqk,bkhd->bqhgd', p[..., :nband], vblk)
                + jnp.einsum('bhgqk,bkhd->bqhgd', p[..., nband:nband + lk], v_ctx))

    o = lax.map(one_block, (qb, bands(k), bands(v), mask))
    return o.transpose(1, 0, 2, 3, 4, 5).reshape(b, s, h * d)


def context_attention(q, k, v, sink=None):
    b, l, h, d = q.shape
    kv = k.shape[2]
    g = h // kv
    lk = k.shape[1]
    qg = q.reshape(b, l, kv, g, d)
    sc = jnp.einsum('bqhgd,bkhd->bhgqk', qg, k).astype(jnp.float32) * ATTN_SCALE
    if sink is not None:
        sk = jnp.broadcast_to(sink.astype(jnp.float32).reshape(kv, g, 1, 1), sc.shape[:-1] + (1,))
        sc = jnp.concatenate([sc, sk], axis=-1)
    p = jax.nn.softmax(sc, axis=-1)[..., :lk].astype(v.dtype)
    o = jnp.einsum('bhgqk,bkhd->bqhgd', p, v)
    return o.reshape(b, l, h * d)


def conformer_conv(u, conv_w, conv_b, ln_g, ln_b):
    a, gt = jnp.split(u, 2, axis=-1)
    hgl = a * jax.nn.sigmoid(gt)
    hc = lax.conv_general_dilated(hgl, conv_w, window_strides=(1,),
                                  padding=[(CONV_W // 2, CONV_W // 2)],
                                  dimension_numbers=('NWC', 'WIO', 'NWC'),
                                  feature_group_count=B_CH) + conv_b
    return jax.nn.silu(layer_norm(hc, ln_g, ln_b))


def token_mixers(hl, hc, w_in, q_g, k_g, conv_w, conv_b, ln_g, ln_b, sink, cos, sin, with_ctx_out):
    aq, ak, av, bu, cq, ck, cv = split_proj(hl @ w_in)
    aqc, akc, avc, buc, cqc, ckc, cvc = split_proj(hc @ w_in)
    ka_c = rms_norm(heads(akc), k_g)
    va_c = heads(avc)
    qa = apply_rope(rms_norm(heads(aq), q_g), cos, sin)
    ka = apply_rope(rms_norm(heads(ak), k_g), cos, sin)
    out_a = global_attention(qa, ka, heads(av), ka_c, va_c)
    out_b = conformer_conv(bu, conv_w, conv_b, ln_g, ln_b)
    kc_c = heads(ckc)
    vc_c = heads(cvc)
    out_c = window_attention(apply_rope(heads(cq), cos, sin), apply_rope(heads(ck), cos, sin),
                             heads(cv), kc_c, vc_c, sink)
    lat = jnp.concatenate([out_a, out_b, out_c], axis=-1)
    if not with_ctx_out:
        return lat, None
    ctx_a = context_attention(rms_norm(heads(aqc), q_g), ka_c, va_c)
    ctx_b = conformer_conv(buc, conv_w, conv_b, ln_g, ln_b)
    ctx_c = context_attention(heads(cqc), kc_c, vc_c, sink)
    return lat, jnp.concatenate([ctx_a, ctx_b, ctx_c], axis=-1)


def hierarchical_moe(h, w_group, b_group, w_expert, b_expert, w_gate, w_up, w_down):
    t, d = h.shape
    gp = jax.nn.softmax((h @ w_group).astype(jnp.float32) + b_group.astype(jnp.float32), axis=-1)
    g_val, g_idx = lax.top_k(gp, 1)
    el = ((h @ w_expert).astype(jnp.float32) + b_expert.astype(jnp.float32)).reshape(t, N_GROUPS, EXPERTS_PER_GROUP)
    el = jnp.take_along_axis(el, g_idx[:, :, None], axis=1)[:, 0]
    e_val, e_loc = lax.top_k(el, TOP_K)
    e_w = jax.nn.softmax(e_val, axis=-1) * g_val
    e_idx = g_idx * EXPERTS_PER_GROUP + e_loc
    n_assign = t * TOP_K
    flat_e = e_idx.reshape(-1)
    order = jnp.argsort(flat_e)
    sorted_e = flat_e[order]
    tok = order // TOP_K
    counts = jnp.bincount(flat_e, length=N_EXPERTS)
    starts = jnp.cumsum(counts) - counts
    padded = (counts + MOE_CHUNK - 1) // MOE_CHUNK * MOE_CHUNK
    pends = jnp.cumsum(padded)
    pstarts = pends - padded
    dest = pstarts[sorted_e] + (jnp.arange(n_assign) - starts[sorted_e])
    n_chunks = -(-n_assign // MOE_CHUNK) + N_EXPERTS
    buf = jnp.zeros((n_chunks * MOE_CHUNK, d), h.dtype).at[dest].set(h[tok])
    chunk_expert = jnp.clip(jnp.searchsorted(pends, jnp.arange(n_chunks) * MOE_CHUNK, side='right'), 0, N_EXPERTS - 1)

    def run(args):
        xb, e = args
        return (jax.nn.silu(xb @ w_gate[e]) * (xb @ w_up[e])) @ w_down[e]

    out = lax.map(run, (buf.reshape(n_chunks, MOE_CHUNK, d), chunk_expert)).reshape(-1, d)
    w_sorted = e_w.reshape(-1)[order].astype(h.dtype)
    return jnp.zeros_like(h).at[tok].add(out[dest] * w_sorted[:, None])


def setup_inputs(seed: int = 0) -> dict:
    key = jax.random.key(seed)
    ks = jax.random.split(key, 32)
    f32 = jnp.float32

    def nrm(k, shape, scale):
        return jax.random.normal(k, shape, f32) * scale

    D = D_MODEL
    return {
        "x": nrm(ks[0], (BATCH, SEQ, D), 1.0),
        "c": nrm(ks[1], (BATCH, D), 1.0),
        "ctx": nrm(ks[2], (BATCH, CTX_LEN, D), 1.0),
        "c_ctx": nrm(ks[3], (D,), 1.0),
        "norm1_g": 1.0 + nrm(ks[4], (DEPTH, D), 0.05),
        "norm2_g": 1.0 + nrm(ks[5], (DEPTH, D), 0.05),
        "w_mod": nrm(ks[6], (DEPTH, D, N_MOD * D), 0.5 * D ** -0.5),
        "b_mod": nrm(ks[7], (DEPTH, N_MOD * D), 0.02),
        "w_in": nrm(ks[8], (DEPTH, D, D_IN), D ** -0.5),
        "q_norm_g": 1.0 + nrm(ks[9], (DEPTH, HEAD_DIM), 0.05),
        "k_norm_g": 1.0 + nrm(ks[10], (DEPTH, HEAD_DIM), 0.05),
        "conv_w": nrm(ks[11], (DEPTH, CONV_W, 1, B_CH), CONV_W ** -0.5),
        "conv_b": nrm(ks[12], (DEPTH, B_CH), 0.02),
        "conv_ln_g": 1.0 + nrm(ks[13], (DEPTH, B_CH), 0.05),
        "conv_ln_b": nrm(ks[14], (DEPTH, B_CH), 0.02),
        "sink": nrm(ks[15], (DEPTH, C_HEADS), 0.5),
        "w_out": nrm(ks[16], (DEPTH, D_MIX, D), D_MIX ** -0.5),
        "w_group": nrm(ks[17], (DEPTH, D, N_GROUPS), D ** -0.5),
        "b_group": nrm(ks[18], (DEPTH, N_GROUPS), 0.01),
        "w_expert": nrm(ks[19], (DEPTH, D, N_EXPERTS), D ** -0.5),
        "b_expert": nrm(ks[20], (DEPTH, N_EXPERTS), 0.01),
        "w_gate": nrm(ks[21], (DEPTH, N_EXPERTS, D, D_EXPERT), D ** -0.5),
        "w_up": nrm(ks[22], (DEPTH, N_EXPERTS, D, D_EXPERT), D ** -0.5),
        "w_down": nrm(ks[23], (DEPTH, N_EXPERTS, D_EXPERT, D), D_EXPERT ** -0.5),
        "final_g": 1.0 + nrm(ks[24], (D,), 0.05),
    }


def reference(x, c, ctx, c_ctx, norm1_g, norm2_g, w_mod, b_mod, w_in, q_norm_g, k_norm_g,
              conv_w, conv_b, conv_ln_g, conv_ln_b, sink, w_out, w_group, b_group,
              w_expert, b_expert, w_gate, w_up, w_down, final_g):
    b, s, d = x.shape
    cos, sin = grid_rope(s)
    xc = ctx
    for i in range(DEPTH):
        with_ctx_out = i < DEPTH - 1
        sh1, sc1, g1, sh2, sc2, g2 = [m[:, None, :] for m in
                                      jnp.split(jax.nn.silu(c) @ w_mod[i] + b_mod[i], N_MOD, axis=-1)]
        csh1, csc1, cg1, csh2, csc2, cg2 = jnp.split(jax.nn.silu(c_ctx) @ w_mod[i] + b_mod[i], N_MOD, axis=-1)
        hl = modulate(rms_norm(x, norm1_g[i]), sh1, sc1)
        hc = modulate(rms_norm(xc, norm1_g[i]), csh1, csc1)
        lat_mix, ctx_mix = token_mixers(hl, hc, w_in[i], q_norm_g[i], k_norm_g[i], conv_w[i], conv_b[i],
                                        conv_ln_g[i], conv_ln_b[i], sink[i], cos, sin, with_ctx_out)
        x = x + g1 * (lat_mix @ w_out[i])
        moe_w = (w_group[i], b_group[i], w_expert[i], b_expert[i], w_gate[i], w_up[i], w_down[i])
        hl = modulate(rms_norm(x, norm2_g[i]), sh2, sc2).reshape(-1, d)
        if with_ctx_out:
            xc = xc + cg1 * (ctx_mix @ w_out[i])
            hc = modulate(rms_norm(xc, norm2_g[i]), csh2, csc2).reshape(-1, d)
            y = hierarchical_moe(jnp.concatenate([hl, hc], axis=0), *moe_w)
            n_lat = hl.shape[0]
            x = x + g2 * y[:n_lat].reshape(x.shape)
            xc = xc + cg2 * y[n_lat:].reshape(xc.shape)
        else:
            x = x + g2 * hierarchical_moe(hl, *moe_w).reshape(x.shape)
    return rms_norm(x, final_g)
```

```python
import contextlib
from contextlib import ExitStack
import numpy as np
import concourse.bass as bass
import concourse.mybir as mybir
from concourse.bass_utils import run_bass_kernel_spmd

F32 = mybir.dt.float32
BF16 = mybir.dt.bfloat16
I32 = mybir.dt.int32
AF = mybir.ActivationFunctionType
ALU = mybir.AluOpType
AX = mybir.AxisListType

SAME_ENG_SYNC = True
COMPUTE = ("pe", "act", "dve", "pool")


class Op:
    __slots__ = ("eng", "fn", "deps", "signal", "dma", "idx", "tok")

    def __init__(self, eng, fn, dma, signal):
        self.eng = eng
        self.fn = fn
        self.deps = []
        self.signal = signal
        self.dma = dma
        self.tok = None


class Sched:
    def __init__(self, nc, es, nslots=None):
        self.nc = nc
        self.es = es
        self.streams = {e: [] for e in ("pe", "act", "dve", "pool", "sp")}
        self.nslots = nslots or {"sp": 8, "act": 4, "pool": 8}
        self.last_w = {}
        self.readers = {}
        self.sb_bytes = 0

    def sb(self, name, shape, dtype):
        n = 1
        for s in shape[1:]:
            n *= s
        self.sb_bytes += n * (2 if dtype == BF16 else 4)
        return self.es.enter_context(self.nc.sbuf_tensor(name, list(shape), dtype))

    def ps(self, name, shape, dtype):
        return self.es.enter_context(self.nc.psum_tensor(name, list(shape), dtype))

    def _target(self, op):
        if op.dma or op.signal:
            return op
        st = self.streams[op.eng]
        for j in range(op.idx + 1, len(st)):
            o = st[j]
            if (not o.dma) and o.signal:
                return o
        op.signal = True
        return op

    def op(self, eng, fn, reads=(), writes=(), dma=False, signal=True):
        o = Op(eng, fn, dma, signal)
        st = self.streams[eng]
        o.idx = len(st)
        deps = []
        for k in reads:
            w = self.last_w.get(k)
            if w is not None:
                deps.append(w)
        for k in writes:
            w = self.last_w.get(k)
            if w is not None:
                deps.append(w)
            deps.extend(self.readers.get(k, ()))
        seen = set()
        for d in deps:
            if d is o:
                continue
            if (not d.dma) and (not dma) and d.eng == eng:
                if eng == "pe" or not SAME_ENG_SYNC:
                    continue
            t = self._target(d)
            if id(t) in seen:
                continue
            seen.add(id(t))
            o.deps.append(t)
        st.append(o)
        for k in writes:
            self.last_w[k] = o
            self.readers[k] = []
        for k in reads:
            if k in writes:
                continue
            lst = self.readers.setdefault(k, [])
            if not dma:
                lst[:] = [r for r in lst if r.dma or r.eng != eng]
            lst.append(o)
        return o

    def emit(self):
        nc, es = self.nc, self.es
        sems = {}
        for e in COMPUTE:
            sems[e] = es.enter_context(nc.semaphore("s_" + e))
        for q, n in self.nslots.items():
            for s in range(n):
                sems[(q, s)] = es.enter_context(nc.semaphore("d_%s%d" % (q, s)))
        for e, st in self.streams.items():
            cnt = 0
            nd = 0
            for o in st:
                if o.dma:
                    n = self.nslots[e]
                    o.tok = ((e, nd % n), 16 * (nd // n + 1))
                    nd += 1
                elif o.signal:
                    cnt += 1
                    o.tok = (e, cnt)
        engmap = {"pe": "tensor", "act": "scalar", "dve": "vector", "pool": "gpsimd", "sp": "sync"}

        def replay(ename, eng):
            known = {}
            for o in self.streams[ename]:
                waits = {}
                for d in o.deps:
                    k, v = d.tok
                    if waits.get(k, 0) < v:
                        waits[k] = v
                if o.dma:
                    k, v = o.tok
                    if v > 16 and waits.get(k, 0) < v - 16:
                        waits[k] = v - 16
                for k, v in waits.items():
                    if known.get(k, 0) >= v:
                        continue
                    known[k] = v
                    eng.wait_ge(sems[k], v)
                ins = o.fn(eng)
                if o.dma:
                    ins.then_inc(sems[o.tok[0]], 16)
                elif o.signal:
                    ins.then_inc(sems[o.tok[0]], 1)
            if ename in self.nslots:
                last = {}
                for o in self.streams[ename]:
                    if o.dma:
                        last[o.tok[0]] = o.tok[1]
                for k, v in last.items():
                    if known.get(k, 0) < v:
                        eng.wait_ge(sems[k], v)

        with nc.Block() as block:
            for ename, attr in engmap.items():
                if not self.streams[ename]:
                    continue

                def mk(ename=ename):
                    def f(eng):
                        replay(ename, eng)
                    return f
                getattr(block, attr)(mk())


D = 1024
L = 256
HD = 64
NE = 32
DE = 512
NMOD = 6
EPS = 1e-6
CONV_W = 31
GRID_W = 64
DIN = 1792
BIG = 1.0e30


class StopBuild(Exception):
    pass


class Cfg:
    def __init__(self, NB=4, S=2048, DEPTH=4, MCH=4, debug=False, stop=None):
        self.NB, self.S, self.DEPTH, self.MCH, self.debug = NB, S, DEPTH, MCH, debug
        self.stop = stop
        self.T = NB * (S + L)
        self.NT = self.T // 128
        self.CH = 128 * MCH
        self.NCH = -(-(2 * self.T) // self.CH) + NE
        self.NSLOT = self.NCH * self.CH
        self.NKB = (S + L) // 128
        self.NQB = S // 128


def host_consts(cfg):
    S = cfg.S
    c = {}
    c["ident"] = np.eye(128, dtype=np.float32)
    sw = np.zeros((128, 128), np.float32)
    for p in range(128):
        sw[p, (p + 64) % 128] = 1.0
    c["swap"] = sw
    bo = np.zeros((128, 128), np.float32)
    bo[:64, :64] = 1.0
    bo[64:, 64:] = 1.0
    c["blockones"] = bo
    RT = np.zeros((128, 128), np.float32)
    for m in range(128):
        dd = m % 64
        half = (dd % 32) // 16
        if half == 0:
            RT[m + 16, m] = -1.0
        else:
            RT[m - 16, m] = 1.0
    c["ropeRT"] = RT
    pos = np.arange(S)
    prow = (pos // GRID_W).astype(np.float32)
    pcol = (pos % GRID_W).astype(np.float32)
    inv = (10000.0 ** (-np.arange(16, dtype=np.float32) / 16)).astype(np.float32)
    cosT = np.zeros((128, S), np.float32)
    sinT = np.zeros((128, S), np.float32)
    for p in range(128):
        dd = p % 64
        seg = dd // 32
        j = dd % 16
        ang = (prow if seg == 0 else pcol) * inv[j]
        cosT[p] = np.cos(ang)
        sinT[p] = np.sin(ang)
    c["cosT"] = cosT
    c["sinT"] = sinT
    kk = np.arange(128)[:, None]
    qq = np.arange(128)[None, :]
    mlo = (qq <= kk).astype(np.float32)
    mhi = (kk <= qq).astype(np.float32)
    c["mlo"] = np.tile(mlo, (1, 3))
    c["mhi"] = np.tile(mhi, (1, 3))
    c["tri"] = (np.arange(128)[:, None] < np.arange(128)[None, :]).astype(np.float32)
    c["jpos"] = np.tile((np.arange(cfg.NCH, dtype=np.float32) * cfg.CH)[None, :], (128, 1))
    c["pidx"] = np.arange(128, dtype=np.float32).reshape(128, 1)
    return c


CONST_SHAPES = None


def build(cfg):
    NB, S, DEPTH, T, NT, NCH, CH, MCH = cfg.NB, cfg.S, cfg.DEPTH, cfg.T, cfg.NT, cfg.NCH, cfg.CH, cfg.MCH
    NBP = NB + 1
    NKB, NQB = cfg.NKB, cfg.NQB
    SL = S + L
    nc = bass.Bass("TRN2", target_bir_lowering=False)

    def din(name, shape, dt=F32):
        return nc.dram_tensor(name, list(shape), dt, kind="ExternalInput").ap()

    x_in = din("x_in", [NB * S, D])
    ctx_in = din("ctx_in", [NB * L, D])
    cT_in = din("cT", [128, 8, NBP])
    n1gT = din("n1gT", [DEPTH, 128, 8])
    n2gT = din("n2gT", [DEPTH, 128, 8])
    w_mod = din("w_mod", [DEPTH, D, NMOD * D])
    bmodT = din("bmodT", [DEPTH, 128, 48])
    b_mod = din("b_mod", [DEPTH, NMOD * D])
    w_in = din("w_in", [DEPTH, D, DIN])
    gqk = din("gqk", [DEPTH, 128, 2])
    convwT = din("convwT", [DEPTH, 128, 2, CONV_W])
    convp = din("convp", [DEPTH, 128, 2, 3])
    sinkb = din("sinkb", [DEPTH, 128, 3])
    w_out = din("w_out", [DEPTH, D, D])
    wr = din("wr", [DEPTH, D, 36])
    br = din("br", [DEPTH, 36])
    w_gate = din("w_gate", [DEPTH * NE * 128, 8 * DE])
    w_up = din("w_up", [DEPTH * NE * 128, 8 * DE])
    w_down = din("w_down", [DEPTH * NE * 128, 4 * D])
    final_g = din("final_g", [D])
    consts = host_consts(cfg)
    cin = {k: din("k_" + k, v.shape) for k, v in consts.items()}
    out = nc.dram_tensor("out", [NB * S, D], F32, kind="ExternalOutput").ap()

    def dscr(name, shape, dt):
        return nc.dram_tensor(name, list(shape), dt, kind="Internal").ap()

    xres = dscr("xres", [T, D], F32)
    h2buf = dscr("h2buf", [T, D], BF16)
    xbuf = dscr("xbuf", [cfg.NSLOT, D], BF16)
    obuf = dscr("obuf", [cfg.NSLOT, D], F32)
    modrows = dscr("modrows", [NBP, 4 * D], F32)
    dbg = {}
    if cfg.debug:
        dbg["xmid"] = nc.dram_tensor("dbg_xmid", [T, D], F32, kind="ExternalOutput").ap()
        dbg["route"] = nc.dram_tensor("dbg_route", [128, NT, 6], F32, kind="ExternalOutput").ap()
        dbg["h2"] = nc.dram_tensor("dbg_h2", [T, D], F32, kind="ExternalOutput").ap()
        dbg["bc"] = nc.dram_tensor("dbg_bc", [3, 128, D], F32, kind="ExternalOutput").ap()
        dbg["ss"] = nc.dram_tensor("dbg_ss", [NT, 128, 4], F32, kind="ExternalOutput").ap()
        dbg["rt"] = nc.dram_tensor("dbg_rt", [NT, 128, 64], F32, kind="ExternalOutput").ap()

    with ExitStack() as es:
        Sx = Sched(nc, es)
        sb, op = Sx.sb, Sx.op

        def dma(q, o, i, reads=(), writes=()):
            op(q, lambda e: e.dma_start(out=o, in_=i), reads=reads, writes=writes, dma=True)

        def mm(o, lhsT, rhs, start, stop, reads, writes, signal=True):
            op("pe", lambda e: e.matmul(o, lhsT=lhsT, rhs=rhs, start=start, stop=stop), reads=reads, writes=writes, signal=signal)

        def tr(o, i, ident, reads, writes, signal=True):
            op("pe", lambda e: e.transpose(out=o, in_=i, identity=ident), reads=reads, writes=writes, signal=signal)

        def act(o, i, func, reads, writes, scale=1.0, bias=0.0, accum_out=None):
            if accum_out is None:
                op("act", lambda e: e.activation(out=o, in_=i, func=func, bias=bias, scale=scale), reads=reads, writes=writes)
            else:
                op("act", lambda e: e.activation(out=o, in_=i, func=func, bias=bias, scale=scale, accum_out=accum_out), reads=reads, writes=writes)

        def tt(o, a, b, alu, reads, writes, eng="dve"):
            op(eng, lambda e: e.tensor_tensor(out=o, in0=a, in1=b, op=alu), reads=reads, writes=writes)

        def ts(o, a, s1, s2, op0, op1, reads, writes, eng="dve"):
            if op1 is None:
                op(eng, lambda e: e.tensor_scalar(out=o, in0=a, scalar1=s1, scalar2=None, op0=op0), reads=reads, writes=writes)
            else:
                op(eng, lambda e: e.tensor_scalar(out=o, in0=a, scalar1=s1, scalar2=s2, op0=op0, op1=op1), reads=reads, writes=writes)

        def stt(o, a, s, b, op0, op1, reads, writes, eng="dve"):
            op(eng, lambda e: e.scalar_tensor_tensor(out=o, in0=a, scalar=s, in1=b, op0=op0, op1=op1), reads=reads, writes=writes)

        def cp(o, i, reads, writes, eng="dve"):
            op(eng, lambda e: e.tensor_copy(out=o, in_=i), reads=reads, writes=writes)

        def recip(o, i, reads, writes):
            op("dve", lambda e: e.reciprocal(out=o, in_=i), reads=reads, writes=writes)

        def red(o, i, alu, reads, writes):
            op("dve", lambda e: e.tensor_reduce(out=o, in_=i, axis=AX.X, op=alu), reads=reads, writes=writes)

        def memset(o, v, writes, eng="dve"):
            op(eng, lambda e: e.memset(o, v), writes=writes)

        banks = [Sx.ps("pb%d" % i, [128, 512], F32) for i in range(8)]

        class Rot:
            def __init__(self, ids):
                self.ids, self.i = ids, 0

            def next(self):
                b = self.ids[self.i % len(self.ids)]
                self.i += 1
                return banks[b], "pb%d" % b

        rot_tr = Rot([0, 1])
        rot_mm = Rot([2, 3])
        rot_acc = Rot([4, 5])
        rot_s = Rot([6, 7, 3])
        rot_d = Rot([6, 7])
        att_ctr = [0]

        def cf32(name, shape):
            t = sb("c_" + name, shape, F32)
            dma("sp", t[:], cin[name], writes=["c_" + name])
            return t

        def cbf(name, shape):
            t = sb("cb_" + name, shape, BF16)
            dma("pool", t[:], cin[name], writes=["cb_" + name])
            return t

        identf = cf32("ident", [128, 128])
        identb = cbf("ident", [128, 128])
        swapf = cf32("swap", [128, 128])
        blockones_b = cbf("blockones", [128, 128])
        ropeRT_b = cbf("ropeRT", [128, 128])
        cosT = cbf("cosT", [128, S])
        sinT = cbf("sinT", [128, S])
        mlo_b = cbf("mlo", [128, 384])
        mhi_b = cbf("mhi", [128, 384])
        trif = cf32("tri", [128, 128])
        jpos = cf32("jpos", [128, NCH])
        pidx = cf32("pidx", [128, 1])
        onesf = sb("onesf", [128, 128], F32)
        memset(onesf[:], 1.0, ["onesf"])
        onesb = sb("onesb", [128, 128], BF16)
        memset(onesb[:], 1.0, ["onesb"])
        zerosf = sb("zerosf", [128, 128], F32)
        memset(zerosf[:], 0.0, ["zerosf"])
        scT = sb("scT", [128, 8, NBP], F32)
        dma("sp", scT[:], cT_in, writes=["scT"])
        act(scT[:], scT[:], AF.Silu, ["scT"], ["scT"])

        wq = sb("wq", [128, 8, 768], BF16)
        wkvo = sb("wkvo", [128, 8, 1024], BF16)
        wrt = sb("wrt", [128, 8, 36], F32)
        brt = sb("brt", [128, 36], F32)
        modT = sb("modT", [128, 48, NBP], F32)
        G2c = sb("G2c", [128, 8, NBP], F32)
        n2T = sb("n2T", [128, 8], F32)
        dg = [sb("dg%d" % i, [128, 128], F32) for i in range(2)]
        bmT = sb("bmT", [128, 48], F32)
        n1T = sb("n1T", [128, 8], F32)
        G1 = sb("G1", [128, 8, NBP], F32)
        gqk_t = sb("gqk_t", [128, 2], F32)
        cw_t = sb("cw_t", [128, 2, CONV_W], F32)
        cpar = sb("cpar", [128, 2, 3], F32)
        diagw = sb("diagw", [128, CONV_W, 128], BF16)
        esk3 = sb("esk3", [128, 3], F32)
        esink_t = sb("esink_t", [128, 3, 128], F32)
        WMC = 256
        g1bc = sb("g1bc", [128, D], F32)
        G2bc = sb("G2bc", [128, D], F32)
        sh2bc = sb("sh2bc", [128, D], F32)
        ARN = max(8 * DE, NKB * 256, 2 * SL, 2 * (S + 30) + 2 * (L + 30))
        AR = [sb("AR%d" % i, [128, ARN], BF16) for i in range(6)]
        VA = AR[0][:, 0:NKB * 256].rearrange("p (k g c) -> p k g c", g=2, c=128)
        VC = AR[1][:, 0:NKB * 256].rearrange("p (k g c) -> p k g c", g=2, c=128)
        kA = AR[2][:, 0:2 * SL].rearrange("p (g t) -> p g t", g=2)
        kC = AR[3][:, 0:2 * SL].rearrange("p (g t) -> p g t", g=2)
        hgl = AR[4][:, 0:2 * (S + 30)].rearrange("p (c t) -> p c t", c=2)
        hglc = AR[4][:, 2 * (S + 30):2 * (S + 30) + 2 * (L + 30)].rearrange("p (c t) -> p c t", c=2)
        cvo = AR[5][:, 0:2 * SL].rearrange("p (c t) -> p c t", c=2)
        wg = [AR[0][:, 0:8 * DE], AR[3][:, 0:8 * DE]]
        wu = [AR[1][:, 0:8 * DE], AR[4][:, 0:8 * DE]]
        wd = [AR[2][:, 0:4 * D], AR[5][:, 0:4 * D]]
        wgk, wuk, wdk = ["AR0", "AR3"], ["AR1", "AR4"], ["AR2", "AR5"]
        wmv = [AR[i][:, 0:4096].bitcast(F32).rearrange("p (c n) -> p c n", c=8) for i in range(4)]
        o12s = [[AR[2 * i + k][:, 0:2048].bitcast(F32) for k in range(2)] for i in range(2)]
        o12sk = [["AR%d" % (2 * i + k) for k in range(2)] for i in range(2)]
        qA = sb("qA", [128, 3, 512], BF16)
        qC = sb("qC", [128, 4, 3, 128], BF16)
        mixc = sb("mixc", [128, 6, 512], BF16)
        xt = [sb("xt%d" % i, [128, D], F32) for i in range(2)]
        xn = sb("xn", [128, 4, D], BF16)
        hT = sb("hT", [128, 8, 512], BF16)
        ss = sb("ss", [128, 4], F32)
        t512 = [sb("t512_%d" % i, [128, 512], F32) for i in range(4)]
        b512 = [sb("b512_%d" % i, [128, 512], BF16) for i in range(3)]
        pT = [sb("pT%d" % i, [128, 512], BF16) for i in range(3)]
        o12 = [xn[:, 2 * k:2 * k + 2, :].rearrange("p a b -> p (a b)").bitcast(F32) for k in range(2)]
        o12k = [[("xn", 0), ("xn", 1)], [("xn", 2), ("xn", 3)]]
        h2T = hT[:, 0:4, :].rearrange("p a b -> p (a b)").bitcast(F32).rearrange("p (c t) -> p c t", t=128)
        h2Tk = [("hT", c) for c in range(4)]
        O1a = sb("O1a", [128, NT, NE], BF16)
        O2a = sb("O2a", [128, NT, NE], BF16)
        r12 = sb("r12", [128, NT, 2], F32)
        w12 = sb("w12", [128, NT, 2], F32)
        dst = sb("dst", [128, NT, 2], I32)
        cum = sb("cum", [128, NE], F32)
        rt = sb("rt", [128, 64], F32)
        rtb = sb("rtb", [128, 4, NE], F32)
        pst = sb("pst", [128, NE + 1], F32)
        pad_i = sb("pad_i", [128, NE], I32)
        cexp = sb("cexp", [128, NCH], F32)
        ctmp = sb("ctmp", [128, NCH], F32)
        gidx = sb("gidx", [128, NCH], I32)
        h2f = sb("h2f", [128, D], F32)
        h2fs = [h2f[:], xn[:, 0:2, :].rearrange("p a b -> p (a b)").bitcast(F32)]
        h2fsk = [["h2f"], [("xn", 0), ("xn", 1)]]
        xb = [sb("xb%d" % i, [128, D], BF16) for i in range(2)]
        xbT2 = [sb("xbT%d" % i, [128, 8, 128], BF16) for i in range(2)]
        a_sb2 = [sb("a_sb%d" % i, [128, DE], BF16) for i in range(2)]
        aT2 = [sb("aT%d" % i, [128, 4, 128], BF16) for i in range(2)]
        ob2 = [h2f, sb("ob1", [128, D], F32)]
        ob2k = ["h2f", "ob1"]

        def segs_of(b):
            return [("lat", b, b * S, S, b), ("ctx", b, NB * S + b * L, L, NB)]

        def src_rows(l, row0, n):
            if l == 0:
                if row0 < NB * S:
                    return x_in[row0:row0 + n, :]
                return ctx_in[row0 - NB * S:row0 - NB * S + n, :]
            return xres[row0:row0 + n, :]

        xti = [0]

        def load_x(l, row0):
            i = xti[0] % 2
            xti[0] += 1
            dma("sp", xt[i][:], src_rows(l, row0, 128), reads=[("xres", row0)], writes=["xt%d" % i])
            return xt[i], "xt%d" % i

        def rstd_of(xtile, xk, col, junk_ap, junk_k):
            act(junk_ap, xtile[:], AF.Square, [xk], list(junk_k) + [("ss", col)], accum_out=ss[:, col:col + 1])
            act(ss[:, col:col + 1], ss[:, col:col + 1], AF.Ln, [("ss", col)], [("ss", col)], scale=1.0 / D, bias=EPS)
            act(ss[:, col:col + 1], ss[:, col:col + 1], AF.Exp, [("ss", col)], [("ss", col)], scale=-0.5)

        def make_hT(l, row0, W, mj):
            ntl = W // 128
            for ti in range(ntl):
                xtile, xk = load_x(l, row0 + ti * 128)
                rstd_of(xtile, xk, ti, xn[:, ti, :], [("xn", ti)])
                ts(xn[:, ti, :], xtile[:], ss[:, ti:ti + 1], None, ALU.mult, None, [xk, ("ss", ti)], [("xn", ti)])
            for c in range(8):
                ps, pk = rot_tr.next()
                psb = ps[:].bitcast(BF16)
                for ti in range(ntl):
                    tr(psb[:, ti * 128:(ti + 1) * 128], xn[:, ti, c * 128:(c + 1) * 128], identb[:], [("xn", ti), "cb_ident"], [pk], signal=(ti == ntl - 1))
                act(hT[:, c, 0:W], psb[:, 0:W], AF.Identity, [pk, "G1", "modT"], [("hT", c)], scale=G1[:, c, mj:mj + 1], bias=modT[:, c, mj:mj + 1])

        hTk = [("hT", c) for c in range(8)]

        def proj(wt, wk, c0, W):
            ps, pk = rot_mm.next()
            for c in range(8):
                mm(ps[:, 0:W], wt[:, c, c0:c0 + 128], hT[:, c, 0:W], c == 0, c == 7, [wk] + hTk, [pk], signal=(c == 7))
            return ps, pk

        def rope_store(src, srck, W, tcols, dst_fn):
            ps, pk = rot_tr.next()
            mm(ps[:, 0:W], ropeRT_b[:], src[:, 0:W], True, True, ["cb_ropeRT", srck], [pk])
            tt(t512[0][:, 0:W], src[:, 0:W], cosT[:, tcols], ALU.mult, [srck, "cb_cosT"], ["t512_0"])
            tt(t512[1][:, 0:W], ps[:, 0:W], sinT[:, tcols], ALU.mult, [pk, "cb_sinT"], ["t512_1"])
            dst_fn(t512[0], t512[1])

        def qk_norm(ps, pk, gcol, W):
            act(b512[0][:, 0:W], ps[:, 0:W], AF.Square, [pk], ["b512_0"])
            p2, p2k = rot_tr.next()
            mm(p2[:, 0:W], blockones_b[:], b512[0][:, 0:W], True, True, ["cb_blockones", "b512_0"], [p2k])
            act(t512[2][:, 0:W], p2[:, 0:W], AF.Ln, [p2k], ["t512_2"], scale=1.0 / HD, bias=EPS)
            act(t512[2][:, 0:W], t512[2][:, 0:W], AF.Exp, ["t512_2"], ["t512_2"], scale=-0.5)
            stt(b512[1][:, 0:W], ps[:, 0:W], gqk_t[:, gcol:gcol + 1], t512[2][:, 0:W], ALU.mult, ALU.mult, [pk, "gqk_t", "t512_2"], ["b512_1"])
            return b512[1], "b512_1"

        def attention(q_ap, qk_, W, keyblocks, kT, kTk, V, Vk, dst_top, dst_bot, sink):
            nk = len(keyblocks)
            steps = [(g, idx) for g in range(2) for idx in range(nk)]
            accs = [rot_acc.next(), rot_acc.next()]
            LA = 2

            def qk_mm(sidx):
                g, idx = steps[sidx]
                kb = keyblocks[idx][0]
                ps, pk = rot_s.next()
                mm(ps[:, 0:W], kT[:, g, kb * 128:(kb + 1) * 128], q_ap, True, True, [kTk, qk_], [pk])
                return ps, pk

            issued = [qk_mm(i) for i in range(min(LA, len(steps)))]
            for sidx, (g, idx) in enumerate(steps):
                kb, mask, mk_ = keyblocks[idx]
                ps, pk = issued[sidx]
                acc, ak = accs[g]
                pi = att_ctr[0] % len(pT)
                att_ctr[0] += 1
                act(pT[pi][:, 0:W], ps[:, 0:W], AF.Exp, [pk], ["pT%d" % pi], scale=HD ** -0.5)
                if mask is not None:
                    tt(pT[pi][:, 0:W], pT[pi][:, 0:W], mask[:, 0:W], ALU.mult, ["pT%d" % pi, mk_], ["pT%d" % pi])
                mm(acc[:, 0:W], V[:, kb, g, :], pT[pi][:, 0:W], idx == 0, idx == nk - 1, [Vk, "pT%d" % pi], [ak], signal=(idx == nk - 1))
                if sidx + LA < len(steps):
                    issued.append(qk_mm(sidx + LA))
            (a0, a0k), (a1, a1k) = accs
            R = t512[2]
            act(R[64:128, 0:W], a0[64:128, 0:W], AF.Identity, [a0k], ["t512_2"])
            act(R[0:64, 0:W], a1[0:64, 0:W], AF.Identity, [a1k], ["t512_2"])
            ps, pk = rot_s.next()
            mm(ps[:, 0:W], swapf[:], R[:, 0:W], True, True, ["c_swap", "t512_2"], [pk])
            if sink:
                tt(t512[3][:, 0:W], ps[:, 0:W], esink_t[:].rearrange("p a b -> p (a b)")[:, 0:W], ALU.add, [pk, "esink_t"], ["t512_3"])
                act(t512[3][:, 0:W], t512[3][:, 0:W], AF.Ln, ["t512_3"], ["t512_3"])
            else:
                act(t512[3][:, 0:W], ps[:, 0:W], AF.Ln, [pk], ["t512_3"])
            act(t512[3][:, 0:W], t512[3][:, 0:W], AF.Exp, ["t512_3"], ["t512_3"], scale=-1.0)
            dst_top(a0, a0k, t512[3])
            dst_bot(a1, a1k, t512[3])

        dgi = [0]

        def build_bc(dst_t, dk, src_t, srck, c_off, j):
            for half in range(2):
                ps, pk = rot_mm.next()
                for c4 in range(4):
                    c = half * 4 + c4
                    di = dgi[0] % 2
                    dgi[0] += 1
                    ts(dg[di][:], identf[:], src_t[:, c_off + c, j:j + 1], None, ALU.mult, None, ["c_ident", srck], ["dg%d" % di])
                    mm(ps[:, c4 * 128:(c4 + 1) * 128], onesf[:], dg[di][:], True, True, ["onesf", "dg%d" % di], [pk])
                act(dst_t[:, half * 512:(half + 1) * 512], ps[:, :], AF.Identity, [pk], [dk])

        def stop_at(name):
            if cfg.stop == name:
                raise StopBuild()

        try:
            for l in range(DEPTH):
                last = (l == DEPTH - 1)
                w_in_v = w_in[l].rearrange("(c p) n -> p c n", p=128)
                dma("pool", wq[:], w_in_v[:, :, 0:768], writes=["wq"])
                dma("sp", wrt[:], wr[l].rearrange("(c p) n -> p c n", p=128), writes=["wrt"])
                dma("sp", brt[:], br[l].partition_broadcast(128), writes=["brt"])
                dma("sp", bmT[:], bmodT[l], writes=["bmT"])
                dma("sp", n1T[:], n1gT[l], writes=["n1T"])
                dma("sp", n2T[:], n2gT[l], writes=["n2T"])
                dma("sp", gqk_t[:], gqk[l], writes=["gqk_t"])
                dma("sp", cw_t[:], convwT[l], writes=["cw_t"])
                dma("sp", cpar[:], convp[l], writes=["cpar"])
                dma("sp", esk3[:], sinkb[l], writes=["esk3"])
                act(esk3[:], esk3[:], AF.Exp, ["esk3"], ["esk3"])
                for hh in range(3):
                    ts(esink_t[:, hh, :], zerosf[:, :], esk3[:, hh:hh + 1], None, ALU.add, None, ["zerosf", "esk3"], ["esink_t"])
                stop_at("params")
                stop_at("arena")
                wmod_v = w_mod[l].rearrange("(c p) n -> p c n", p=128)
                npieces = (6 * D) // WMC
                for piece in range(npieces):
                    wt_, wk = wmv[piece % 4], "AR%d" % (piece % 4)
                    dma("sp", wt_, wmod_v[:, :, piece * WMC:(piece + 1) * WMC], writes=[wk])
                    for f in range(WMC // 128):
                        fc = piece * (WMC // 128) + f
                        ps, pk = rot_mm.next()
                        for c in range(8):
                            mm(ps[:, 0:NBP], wt_[:, c, f * 128:(f + 1) * 128], scT[:, c, :], c == 0, c == 7, [wk, "scT"], [pk], signal=(c == 7))
                        act(modT[:, fc, :], ps[:, 0:NBP], AF.Identity, [pk, "bmT"], ["modT"], bias=bmT[:, fc:fc + 1])
                for c in range(8):
                    ts(G1[:, c, :], modT[:, 8 + c, :], 1.0, n1T[:, c:c + 1], ALU.add, ALU.mult, ["modT", "n1T"], ["G1"])
                    ts(G2c[:, c, :], modT[:, 32 + c, :], 1.0, n2T[:, c:c + 1], ALU.add, ALU.mult, ["modT", "n2T"], ["G2c"])

                memset(AR[0][:], 1.0, ["AR0"])
                memset(AR[1][:], 1.0, ["AR1"])
                memset(AR[2][:], 0.0, ["AR2"])
                memset(AR[3][:], 0.0, ["AR3"])
                memset(AR[4][:], 0.0, ["AR4"], eng="pool")
                stop_at("mod")
                memset(cum[:], 0.0, ["cum"])
                tile_ctr = [0]
                moe_tiles = []
                for b in range(NB):
                    dma("pool", wkvo[:], w_in_v[:, :, 768:DIN], reads=[], writes=["wkvo"])
                    for (kind, _, row0, ntok, mj) in segs_of(b):
                        is_lat = kind == "lat"
                        col_base = 0 if is_lat else S
                        for g0 in range(0, ntok, 512):
                            W = min(512, ntok - g0)
                            ntl = W // 128
                            make_hT(l, row0 + g0, W, mj)
                            cols = slice(col_base + g0, col_base + g0 + W)
                            tcols = slice(g0, g0 + W)
                            nb0 = (col_base + g0) // 128
                            stop_at("hT")
                            ps, pk = proj(wkvo, "wkvo", 0, W)
                            kn, knk = qk_norm(ps, pk, 1, W)
                            if is_lat:
                                def kst(a, bb, cols=cols, W=W):
                                    tt(kA[0:64, 0, cols], a[0:64, 0:W], bb[0:64, 0:W], ALU.add, ["t512_0", "t512_1"], ["AR2"])
                                    tt(kA[64:128, 1, cols], a[64:128, 0:W], bb[64:128, 0:W], ALU.add, ["t512_0", "t512_1"], ["AR2"])
                                rope_store(kn, knk, W, tcols, kst)
                            else:
                                cp(kA[0:64, 0, cols], kn[0:64, 0:W], [knk], ["AR2"])
                                cp(kA[64:128, 1, cols], kn[64:128, 0:W], [knk], ["AR2"])
                            stop_at("ak")
                            hg = hgl if is_lat else hglc
                            for cc in range(2):
                                psg, pgk = proj(wkvo, "wkvo", (3 + cc) * 128, W)
                                act(t512[3][:, 0:W], psg[:, 0:W], AF.Sigmoid, [pgk], ["t512_3"])
                                psa, pak = proj(wkvo, "wkvo", (1 + cc) * 128, W)
                                tt(hg[:, cc, 15 + g0:15 + g0 + W], psa[:, 0:W], t512[3][:, 0:W], ALU.mult, [pak, "t512_3"], ["AR4"])
                            stop_at("glu")
                            ps, pk = proj(wkvo, "wkvo", 5 * 128, W)
                            if is_lat:
                                act(b512[2][:, 0:W], ps[:, 0:W], AF.Identity, [pk], ["b512_2"])

                                def kcst(a, bb, cols=cols, W=W):
                                    tt(kC[0:64, 0, cols], a[0:64, 0:W], bb[0:64, 0:W], ALU.add, ["t512_0", "t512_1"], ["AR3"])
                                    tt(kC[64:128, 1, cols], a[64:128, 0:W], bb[64:128, 0:W], ALU.add, ["t512_0", "t512_1"], ["AR3"])
                                rope_store(b512[2], "b512_2", W, tcols, kcst)
                            else:
                                act(kC[0:64, 0, cols], ps[0:64, 0:W], AF.Identity, [pk], ["AR3"])
                                act(kC[64:128, 1, cols], ps[64:128, 0:W], AF.Identity, [pk], ["AR3"])
                            stop_at("ck")
                            for ti in range(ntl):
                                kb = nb0 + ti
                                ps, pk = rot_mm.next()
                                for c in range(8):
                                    mm(ps[:, 0:256], hT[:, c, ti * 128:(ti + 1) * 128], wkvo[:, c, 768:1024], c == 0, c == 7, ["wkvo"] + hTk, [pk], signal=(c == 7))
                                cp(VA[:, kb, 0, 0:64], ps[:, 0:64], [pk], ["AR0"])
                                cp(VA[:, kb, 1, 64:128], ps[:, 64:128], [pk], ["AR0"])
                                cp(VC[:, kb, 0, 0:64], ps[:, 128:192], [pk], ["AR1"])
                                cp(VC[:, kb, 1, 64:128], ps[:, 192:256], [pk], ["AR1"])
                            stop_at("v1")
                    stop_at("pass1")
                    dma("pool", wkvo[:], w_out[l].rearrange("(c p) n -> p c n", p=128), writes=["wkvo"])
                    conv_segs = [(hgl, S, 0)] + ([] if last else [(hglc, L, S)])
                    for cc in range(2):
                        for j in range(CONV_W):
                            ts(diagw[:, j, :], identf[:, :], cw_t[:, cc, j:j + 1], None, ALU.mult, None, ["c_ident", "cw_t"], ["diagw"])
                        for (hg, n_tok, cb) in conv_segs:
                            for g0 in range(0, n_tok, 512):
                                W = min(512, n_tok - g0)
                                ps, pk = rot_mm.next()
                                for j in range(CONV_W):
                                    mm(ps[:, 0:W], diagw[:, j, :], hg[:, cc, g0 + j:g0 + j + W], j == 0, j == CONV_W - 1, ["diagw", "AR4"], [pk], signal=(j == CONV_W - 1))
                                act(cvo[:, cc, cb + g0:cb + g0 + W], ps[:, 0:W], AF.Identity, [pk, "cpar"], ["AR5"], bias=cpar[:, cc, 0:1])
                    for (hg, n_tok, cb) in conv_segs:
                        for g0 in range(0, n_tok, 512):
                            W = min(512, n_tok - g0)
                            cs = slice(cb + g0, cb + g0 + W)
                            p1, p1k = rot_tr.next()
                            for cc in range(2):
                                mm(p1[:, 0:W], onesb[:], cvo[:, cc, cs], cc == 0, cc == 1, ["onesb", "AR5"], [p1k], signal=(cc == 1))
                            act(t512[0][:, 0:W], p1[:, 0:W], AF.Identity, [p1k], ["t512_0"], scale=1.0 / 256)
                            p2, p2k = rot_tr.next()
                            for cc in range(2):
                                act(b512[cc][:, 0:W], cvo[:, cc, cs], AF.Square, ["AR5"], ["b512_%d" % cc])
                                mm(p2[:, 0:W], onesb[:], b512[cc][:, 0:W], cc == 0, cc == 1, ["onesb", "b512_%d" % cc], [p2k], signal=(cc == 1))
                            tt(t512[1][:, 0:W], t512[0][:, 0:W], t512[0][:, 0:W], ALU.mult, ["t512_0"], ["t512_1"])
                            stt(t512[2][:, 0:W], p2[:, 0:W], 1.0 / 256, t512[1][:, 0:W], ALU.mult, ALU.subtract, [p2k, "t512_1"], ["t512_2"])
                            act(t512[2][:, 0:W], t512[2][:, 0:W], AF.Ln, ["t512_2"], ["t512_2"], bias=EPS)
                            act(t512[2][:, 0:W], t512[2][:, 0:W], AF.Exp, ["t512_2"], ["t512_2"], scale=-0.5)
                            for cc in range(2):
                                tt(t512[3][:, 0:W], cvo[:, cc, cs], t512[0][:, 0:W], ALU.subtract, ["AR5", "t512_0"], ["t512_3"])
                                tt(t512[3][:, 0:W], t512[3][:, 0:W], t512[2][:, 0:W], ALU.mult, ["t512_3", "t512_2"], ["t512_3"])
                                act(cvo[:, cc, cs], t512[3][:, 0:W], AF.Silu, ["t512_3", "cpar"], ["AR5"], scale=cpar[:, cc, 1:2], bias=cpar[:, cc, 2:3])

                    stop_at("conv")
                    ctxkeys = [(NQB, None, None), (NQB + 1, None, None)]
                    allkeys = [(kb, None, None) for kb in range(NKB)]
                    for (kind, _, row0, ntok, mj) in segs_of(b):
                        is_lat = kind == "lat"
                        if last and not is_lat:
                            continue
                        col_base = 0 if is_lat else S
                        build_bc(g1bc, "g1bc", modT, "modT", 16, mj)
                        build_bc(sh2bc, "sh2bc", modT, "modT", 24, mj)
                        build_bc(G2bc, "G2bc", G2c, "G2c", 0, mj)
                        if cfg.debug and l == 0 and b == 0 and is_lat:
                            dma("sp", dbg["bc"][0], g1bc[:], reads=["g1bc"])
                            dma("sp", dbg["bc"][1], sh2bc[:], reads=["sh2bc"])
                            dma("sp", dbg["bc"][2], G2bc[:], reads=["G2bc"])
                        for g0 in range(0, ntok, 512):
                            W = min(512, ntok - g0)
                            ntl = W // 128
                            make_hT(l, row0 + g0, W, mj)
                            tcols = slice(g0, g0 + W)
                            for i in range(3):
                                ps, pk = proj(wq, "wq", i * 128, W)
                                qn, qnk = qk_norm(ps, pk, 0, W)
                                if is_lat:
                                    rope_store(qn, qnk, W, tcols, lambda a, bb, i=i, W=W: tt(qA[:, i, 0:W], a[:, 0:W], bb[:, 0:W], ALU.add, ["t512_0", "t512_1"], ["qA"]))
                                else:
                                    cp(qA[:, i, 0:W], qn[:, 0:W], [qnk], ["qA"])
                            for i in range(3):
                                ps, pk = proj(wq, "wq", (3 + i) * 128, W)
                                if is_lat:
                                    act(b512[2][:, 0:W], ps[:, 0:W], AF.Identity, [pk], ["b512_2"])
                                    rope_store(b512[2], "b512_2", W, tcols, lambda a, bb, i=i, W=W, ntl=ntl: tt(
                                        qC[:, 0:ntl, i, :], a[:, 0:W].rearrange("p (n q) -> p n q", q=128), bb[:, 0:W].rearrange("p (n q) -> p n q", q=128),
                                        ALU.add, ["t512_0", "t512_1"], ["qC"]))
                                else:
                                    act(qC[:, 0:ntl, i, :], ps[:, 0:W].rearrange("p (n q) -> p n q", q=128), AF.Identity, [pk], ["qC"])
                            keysA = allkeys if is_lat else ctxkeys
                            for i in range(3):
                                attention(qA[:, i, 0:W], "qA", W, keysA, kA, "AR2", VA, "AR0",
                                          lambda a, ak, rc, i=i, W=W: tt(mixc[0:64, i, 0:W], a[0:64, 0:W], rc[0:64, 0:W], ALU.mult, [ak, "t512_3"], ["mixc"]),
                                          lambda a, ak, rc, i=i, W=W: tt(mixc[64:128, i, 0:W], a[64:128, 0:W], rc[64:128, 0:W], ALU.mult, [ak, "t512_3"], ["mixc"]),
                                          False)
                            def attn_c(bi, g0=g0, is_lat=is_lat):
                                if is_lat:
                                    n = g0 // 128 + bi
                                    kbs = []
                                    if n > 0:
                                        kbs.append((n - 1, mlo_b, "cb_mlo"))
                                    kbs.append((n, None, None))
                                    if n < NQB - 1:
                                        kbs.append((n + 1, mhi_b, "cb_mhi"))
                                    kbs += ctxkeys
                                else:
                                    kbs = ctxkeys
                                c0 = bi * 128
                                attention(qC[:, bi, :, :].rearrange("p a b -> p (a b)"), "qC", 384, kbs, kC, "AR3", VC, "AR1",
                                          lambda a, ak, rc, c0=c0: tt(mixc[0:64, 3:6, c0:c0 + 128], a[0:64, 0:384].rearrange("p (a b) -> p a b", b=128),
                                                                      rc[0:64, 0:384].rearrange("p (a b) -> p a b", b=128), ALU.mult, [ak, "t512_3"], [("mixcC", c0)]),
                                          lambda a, ak, rc, c0=c0: tt(mixc[64:128, 3:6, c0:c0 + 128], a[64:128, 0:384].rearrange("p (a b) -> p a b", b=128),
                                                                      rc[64:128, 0:384].rearrange("p (a b) -> p a b", b=128), ALU.mult, [ak, "t512_3"], [("mixcC", c0)]),
                                          True)
                            pre = {0: load_x(l, row0 + g0)}

                            def tile_gen(tl, pre=pre, row0=row0, g0=g0, ntl=ntl, col_base=col_base, mj=mj):
                                r0 = row0 + g0 + tl * 128
                                cg = col_base + g0 + tl * 128
                                xtile, xk = pre[tl]
                                if tl + 1 < ntl:
                                    pre[tl + 1] = load_x(l, r0 + 128)
                                hb, hbk = h2fs[tl % 2], h2fsk[tl % 2]
                                sc_ = tl % 4
                                attn_c(tl)
                                ck_ = ("mixcC", tl * 128)
                                lhs = [(mixc[:, 0, tl * 128:(tl + 1) * 128], "mixc"), (mixc[:, 1, tl * 128:(tl + 1) * 128], "mixc"), (mixc[:, 2, tl * 128:(tl + 1) * 128], "mixc"),
                                       (cvo[:, 0, cg:cg + 128], "AR5"), (cvo[:, 1, cg:cg + 128], "AR5"),
                                       (mixc[:, 3, tl * 128:(tl + 1) * 128], ck_), (mixc[:, 4, tl * 128:(tl + 1) * 128], ck_), (mixc[:, 5, tl * 128:(tl + 1) * 128], ck_)]
                                for half in range(2):
                                    hs = slice(half * 512, (half + 1) * 512)
                                    ps, pk = rot_mm.next()
                                    for c in range(8):
                                        mm(ps[:, :], lhs[c][0], wkvo[:, c, hs], c == 0, c == 7, [lhs[c][1], "wkvo"], [pk], signal=(c == 7))
                                    tt(t512[half][:, :], ps[:, :], g1bc[:, hs], ALU.mult, [pk, "g1bc"], ["t512_%d" % half])
                                    tt(xtile[:, hs], xtile[:, hs], t512[half][:, :], ALU.add, [xk, "t512_%d" % half], [xk])
                                dma("sp", xres[r0:r0 + 128, :], xtile[:], reads=[xk], writes=[("xres", r0)])
                                if cfg.debug and l == 0:
                                    dma("sp", dbg["xmid"][r0:r0 + 128, :], xtile[:], reads=[xk])
                                rstd_of(xtile, xk, sc_, hb, hbk)
                                stt(hb, xtile[:], ss[:, sc_:sc_ + 1], G2bc[:], ALU.mult, ALU.mult, [xk, ("ss", sc_), "G2bc"], hbk)
                                tt(hb, hb, sh2bc[:], ALU.add, hbk + ["sh2bc"], hbk)
                                yield
                                ti = tile_ctr[0]
                                tile_ctr[0] += 1
                                moe_tiles.append((r0, mj))
                                act(xb[0][:], hb, AF.Identity, hbk, ["xb0"])
                                if cfg.debug and l == 0:
                                    dma("sp", dbg["h2"][r0:r0 + 128, :], hb, reads=hbk)
                                    dma("sp", dbg["ss"][ti], ss[:, :], reads=[("ss", sc_)])
                                dma("sp", h2buf[r0:r0 + 128, :], xb[0][:], reads=["xb0"], writes=[("h2buf", r0)])
                                for hf in range(2):
                                    ps, pk = rot_tr.next()
                                    for c4 in range(4):
                                        c = hf * 4 + c4
                                        tr(ps[:, c4 * 128:(c4 + 1) * 128], hb[:, c * 128:(c + 1) * 128], identf[:], hbk + ["c_ident"], [pk], signal=(c4 == 3))
                                    act(h2T[:, hf * 4:hf * 4 + 4, :], ps[:, :].rearrange("p (a b) -> p a b", b=128), AF.Identity, [pk], h2Tk)
                                ps, pk = rot_mm.next()
                                for c in range(8):
                                    mm(ps[:, 0:36], h2T[:, c, :], wrt[:, c, :], c == 0, c == 7, h2Tk + ["wrt"], [pk], signal=(c == 7))
                                RT_ = "rt"
                                tt(rt[:, 0:36], ps[:, 0:36], brt[:, :], ALU.add, [pk, "brt"], [RT_])
                                red(rt[:, 36:37], rt[:, 0:4], ALU.max, [RT_], [RT_])
                                ts(rt[:, 48:49], rt[:, 36:37], -1.0, None, ALU.mult, None, [RT_], [RT_])
                                act(rt[:, 40:44], rt[:, 0:4], AF.Exp, [RT_], [RT_], bias=rt[:, 48:49], accum_out=rt[:, 37:38])
                                recip(rt[:, 37:38], rt[:, 37:38], [RT_], [RT_])
                                ts(rt[:, 40:44], rt[:, 0:4], rt[:, 36:37], None, ALU.is_equal, None, [RT_], [RT_])
                                ts(rt[:, 44:48], rt[:, 40:44], -1.0, BIG, ALU.add, ALU.mult, [RT_], [RT_])
                                for g in range(4):
                                    ts(rtb[:, 0, g * 8:(g + 1) * 8], rt[:, 4 + g * 8:12 + g * 8], rt[:, 44 + g:45 + g], None, ALU.add, None, [RT_], ["rtb"])
                                red(rt[:, 38:39], rtb[:, 0, :], ALU.max, ["rtb"], [RT_])
                                ts(O1a[:, ti, :], rtb[:, 0, :], rt[:, 38:39], None, ALU.is_equal, None, ["rtb", RT_], [("O1", ti)])
                                stt(rtb[:, 1, :], O1a[:, ti, :], -BIG, rtb[:, 0, :], ALU.mult, ALU.add, [("O1", ti), "rtb"], ["rtb"])
                                red(rt[:, 39:40], rtb[:, 1, :], ALU.max, ["rtb"], [RT_])
                                ts(O2a[:, ti, :], rtb[:, 1, :], rt[:, 39:40], None, ALU.is_equal, None, ["rtb", RT_], [("O2", ti)])
                                tt(rt[:, 48:49], rt[:, 39:40], rt[:, 38:39], ALU.subtract, [RT_], [RT_])
                                act(rt[:, 48:49], rt[:, 48:49], AF.Exp, [RT_], [RT_])
                                ts(rt[:, 48:49], rt[:, 48:49], 1.0, None, ALU.add, None, [RT_], [RT_])
                                recip(rt[:, 48:49], rt[:, 48:49], [RT_], [RT_])
                                tt(w12[:, ti, 0:1], rt[:, 48:49], rt[:, 37:38], ALU.mult, [RT_], [("w12", ti)])
                                tt(w12[:, ti, 1:2], rt[:, 37:38], w12[:, ti, 0:1], ALU.subtract, [RT_, ("w12", ti)], [("w12", ti)])
                                tt(rtb[:, 2, :], O1a[:, ti, :], O2a[:, ti, :], ALU.add, [("O1", ti), ("O2", ti)], ["rtb"])
                                ps, pk = rot_mm.next()
                                mm(ps[:, 0:NE], trif[:], rtb[:, 2, :], True, True, ["c_tri", "rtb"], [pk])
                                tt(rtb[:, 3, :], ps[:, 0:NE], cum[:], ALU.add, [pk, "cum"], ["rtb"])
                                ps2, pk2 = rot_mm.next()
                                mm(ps2[:, 0:NE], onesf[:], rtb[:, 2, :], True, True, ["onesf", "rtb"], [pk2])
                                tt(cum[:], cum[:], ps2[:, 0:NE], ALU.add, ["cum", pk2], ["cum"])
                                tt(rtb[:, 0, :], O1a[:, ti, :], rtb[:, 3, :], ALU.mult, [("O1", ti), "rtb"], ["rtb"])
                                red(r12[:, ti, 0:1], rtb[:, 0, :], ALU.add, ["rtb"], [("r12", ti)])
                                tt(rtb[:, 1, :], O2a[:, ti, :], rtb[:, 3, :], ALU.mult, [("O2", ti), "rtb"], ["rtb"])
                                red(r12[:, ti, 1:2], rtb[:, 1, :], ALU.add, ["rtb"], [("r12", ti)])
                                if cfg.debug and l == 0:
                                    dma("sp", dbg["rt"][ti], rt[:, :], reads=["rt"])

                            gens = [tile_gen(tl) for tl in range(ntl)]
                            next(gens[0])
                            for tl in range(ntl):
                                if tl + 1 < ntl:
                                    next(gens[tl + 1])
                                for _ in gens[tl]:
                                    pass

                stop_at("pass2")
                nmt = tile_ctr[0]
                ts(rtb[:, 0, :], cum[:], float(CH - 1), None, ALU.add, None, ["cum"], ["rtb"])
                cp(pad_i[:], rtb[:, 0, :], ["rtb"], ["pad_i"])
                sh = int(np.log2(CH))
                ts(pad_i[:], pad_i[:], sh, None, ALU.arith_shift_right, None, ["pad_i"], ["pad_i"])
                ts(pad_i[:], pad_i[:], sh, None, ALU.logical_shift_left, None, ["pad_i"], ["pad_i"])
                cp(rtb[:, 1, :], pad_i[:], ["pad_i"], ["rtb"])
                memset(pst[:, 0:1], 0.0, ["pst"])
                for e in range(NE):
                    tt(pst[:, e + 1:e + 2], pst[:, e:e + 1], rtb[:, 1, e:e + 1], ALU.add, ["pst", "rtb"], ["pst"])
                memset(cexp[:], 0.0, ["cexp"])
                for e in range(NE):
                    ts(ctmp[:], jpos[:], pst[:, e + 1:e + 2], None, ALU.is_ge, None, ["c_jpos", "pst"], ["ctmp"])
                    tt(cexp[:], cexp[:], ctmp[:], ALU.add, ["cexp", "ctmp"], ["cexp"])
                ts(cexp[:], cexp[:], float(NE - 1), float(l * NE), ALU.min, ALU.add, ["cexp"], ["cexp"])
                ts(ctmp[:], cexp[:], 128.0, pidx[:, 0:1], ALU.mult, ALU.add, ["cexp", "c_pidx"], ["ctmp"])
                cp(gidx[:], ctmp[:], ["ctmp"], ["gidx"])
                for ti in range(nmt):
                    for k, Oa, Ok in ((0, O1a, "O1"), (1, O2a, "O2")):
                        tt(rtb[:, k, :], Oa[:, ti, :], pst[:, 0:NE], ALU.mult, [(Ok, ti), "pst"], ["rtb"])
                        red(rt[:, k:k + 1], rtb[:, k, :], ALU.add, ["rtb"], ["rt"])
                    tt(rt[:, 2:4], rt[:, 0:2], r12[:, ti, :], ALU.add, ["rt", ("r12", ti)], ["rt"])
                    cp(dst[:, ti, :], rt[:, 2:4], ["rt"], [("dst", ti)])
                if cfg.debug and l == 0:
                    dbr = sb("dbr", [128, NT, 6], F32)
                    memset(dbr[:], 0.0, ["dbr"])
                    for ti in range(nmt):
                        cp(dbr[:, ti, 0:2], dst[:, ti, :], [("dst", ti)], ["dbr"])
                        cp(dbr[:, ti, 2:4], w12[:, ti, :], [("w12", ti)], ["dbr"])
                        cp(dbr[:, ti, 4:6], r12[:, ti, :], [("r12", ti)], ["dbr"])
                    dma("sp", dbg["route"], dbr[:], reads=["dbr"])
                stop_at("disp")
                xbw_keys = [("xbw", n) for n in range(2 * nmt)]
                for ti, (r0, mj) in enumerate(moe_tiles):
                    xbt, xbk = xb[ti % 2], "xb%d" % (ti % 2)
                    dma("sp", xbt[:], h2buf[r0:r0 + 128, :], reads=[("h2buf", r0)], writes=[xbk])
                    for k in range(2):
                        op("pool", lambda e, xbt=xbt, ti=ti, k=k: e.indirect_dma_start(
                            out=xbuf[:, :], out_offset=bass.IndirectOffsetOnAxis(ap=dst[:, ti, k:k + 1], axis=0), in_=xbt[:, :], in_offset=None),
                            reads=[xbk, ("dst", ti)], writes=[("xbw", 2 * ti + k)], dma=True)

                stop_at("scatter")
                nch_used = -(-(2 * nmt * 128) // CH) + NE
                subs = [(j, m) for j in range(nch_used) for m in range(MCH)]

                def stage_a(si):
                    j, m = subs[si]
                    wi = j % 2
                    if m == 0:
                        for (wt_, wname, src) in ((wg[wi], wgk[wi], w_gate), (wu[wi], wuk[wi], w_up), (wd[wi], wdk[wi], w_down)):
                            op("pool", lambda e, wt_=wt_, src=src, j=j: e.indirect_dma_start(
                                out=wt_, out_offset=None, in_=src[:, :], in_offset=bass.IndirectOffsetOnAxis(ap=gidx[:, j:j + 1], axis=0)),
                                reads=["gidx"], writes=[wname], dma=True)
                    s0 = j * CH + m * 128
                    xi = si % 2
                    xbt, xbk = xb[xi], "xb%d" % xi
                    xbT, xbTk = xbT2[xi], "xbT%d" % xi
                    xv = xbt[:].rearrange("s (p c) -> s c p", c=8)
                    for hf in range(2):
                        ps, pk = rot_tr.next()
                        psb = ps[:].bitcast(BF16)
                        for c4 in range(4):
                            c = hf * 4 + c4
                            tr(psb[:, c4 * 128:(c4 + 1) * 128], xv[:, c, :], identb[:], [xbk, "cb_ident"], [pk], signal=(c4 == 3))
                        act(xbT[:, hf * 4:hf * 4 + 4, :], psb[:, 0:512].rearrange("p (a b) -> p a b", b=128), AF.Identity, [pk], [xbTk])

                def stage_b(si):
                    j, m = subs[si]
                    wi = j % 2
                    xi = si % 2
                    xbT, xbTk = xbT2[xi], "xbT%d" % xi
                    psg, pgk = rot_acc.next()
                    for c in range(8):
                        mm(psg[:, :], xbT[:, c, :], wg[wi][:, c * DE:(c + 1) * DE], c == 0, c == 7, [xbTk, wgk[wi]], [pgk], signal=(c == 7))
                    psu, puk = rot_mm.next()
                    for c in range(8):
                        mm(psu[:, :], xbT[:, c, :], wu[wi][:, c * DE:(c + 1) * DE], c == 0, c == 7, [xbTk, wuk[wi]], [puk], signal=(c == 7))
                    return (psg, pgk, psu, puk)

                def stage_c(si, st):
                    j, m = subs[si]
                    wi = j % 2
                    psg, pgk, psu, puk = st
                    s0 = j * CH + m * 128
                    xi = si % 2
                    a_sb, a_sbk = a_sb2[xi], "a_sb%d" % xi
                    aT, aTk = aT2[xi], "aT%d" % xi
                    ob, obk = ob2[xi], ob2k[xi]
                    tsl, tslk = t512[xi], "t512_%d" % xi
                    act(tsl[:, :], psg[:, :], AF.Silu, [pgk], [tslk])
                    tt(a_sb[:, :], tsl[:, :], psu[:, :], ALU.mult, [tslk, puk], [a_sbk])
                    av = a_sb[:].rearrange("s (p c) -> s c p", c=4)
                    ps, pk = rot_tr.next()
                    psb = ps[:].bitcast(BF16)
                    for c in range(4):
                        tr(psb[:, c * 128:(c + 1) * 128], av[:, c, :], identb[:], [a_sbk, "cb_ident"], [pk], signal=(c == 3))
                    cp(aT[:], psb[:, 0:512].rearrange("p (a b) -> p a b", b=128), [pk], [aTk])

                def stage_c2(si):
                    j, m = subs[si]
                    wi = j % 2
                    s0 = j * CH + m * 128
                    xi = si % 2
                    aT, aTk = aT2[xi], "aT%d" % xi
                    ob, obk = ob2[xi], ob2k[xi]
                    for half in range(2):
                        ps, pk = rot_d.next()
                        for c in range(4):
                            mm(ps[:, :], aT[:, c, :], wd[wi][:, c * D + half * 512:c * D + half * 512 + 512], c == 0, c == 3, [aTk, wdk[wi]], [pk], signal=(c == 3))
                        if half == 0:
                            act(ob[:, 0:512], ps[:, :], AF.Identity, [pk], [obk])
                        else:
                            cp(ob[:, 512:1024], ps[:, :], [pk], [obk])
                    dma("sp", obuf[s0:s0 + 128, :], ob[:], reads=[obk], writes=["obuf"])

                def load_sub(si):
                    j, m = subs[si]
                    s0 = j * CH + m * 128
                    dma("sp", xb[si % 2][:], xbuf[s0:s0 + 128, :], reads=xbw_keys, writes=["xb%d" % (si % 2)])

                assert MCH >= 3
                nsub = len(subs)
                load_sub(0)
                load_sub(1)
                stage_a(0)
                load_sub(2)
                stage_a(1)
                st_next = stage_b(0)
                for si in range(nsub):
                    st_cur = st_next
                    if si + 2 < nsub:
                        stage_a(si + 2)
                    if si + 3 < nsub:
                        load_sub(si + 3)
                    stage_c(si, st_cur)
                    if si + 1 < nsub:
                        st_next = stage_b(si + 1)
                    stage_c2(si)

                stop_at("experts")
                if last:
                    dma("sp", sh2bc[:], final_g.partition_broadcast(128), writes=["sh2bc"])
                cur_mj = None
                for ti, (r0, mj) in enumerate(moe_tiles):
                    if mj != cur_mj:
                        build_bc(g1bc, "g1bc", modT, "modT", 40, mj)
                        cur_mj = mj
                    o12c, o12ck = o12s[ti % 2], o12sk[ti % 2]
                    for k in range(2):
                        op("pool", lambda e, ti=ti, k=k, o12c=o12c: e.indirect_dma_start(
                            out=o12c[k], out_offset=None, in_=obuf[:, :], in_offset=bass.IndirectOffsetOnAxis(ap=dst[:, ti, k:k + 1], axis=0)),
                            reads=["obuf", ("dst", ti)], writes=[o12ck[k]], dma=True)
                    if ti == 0:
                        cpre = load_x(1, r0)
                    xtile, xk = cpre
                    if ti + 1 < len(moe_tiles):
                        cpre = load_x(1, moe_tiles[ti + 1][0])
                    act(o12c[0], o12c[0], AF.Identity, [o12ck[0], ("w12", ti)], [o12ck[0]], scale=w12[:, ti, 0:1])
                    stt(o12c[0], o12c[1], w12[:, ti, 1:2], o12c[0], ALU.mult, ALU.add, [o12ck[1], o12ck[0], ("w12", ti)], [o12ck[0]])
                    tt(o12c[0], o12c[0], g1bc[:], ALU.mult, [o12ck[0], "g1bc"], [o12ck[0]])
                    tt(xtile[:], xtile[:], o12c[0], ALU.add, [xk, o12ck[0]], [xk])
                    if not last:
                        dma("sp", xres[r0:r0 + 128, :], xtile[:], reads=[xk], writes=[("xres", r0)])
                    else:
                        rstd_of(xtile, xk, 0, h2f[:], ["h2f"])
                        stt(xtile[:], xtile[:], ss[:, 0:1], sh2bc[:], ALU.mult, ALU.mult, [xk, ("ss", 0), "sh2bc"], [xk])
                        dma("sp", out[r0:r0 + 128, :], xtile[:], reads=[xk])
        except StopBuild:
            pass
        Sx.emit()
        build.sb_bytes = Sx.sb_bytes
        build.counts = {e: len(s) for e, s in Sx.streams.items()}
    return nc


def _perm_in():
    aq, ak, av, bu, cq, ck, cv = 0, 384, 512, 640, 1152, 1536, 1664
    cols = []
    for i in range(3):
        cols += list(range(aq + i * 64, aq + i * 64 + 64)) + list(range(aq + (i + 3) * 64, aq + (i + 3) * 64 + 64))
    for i in range(3):
        cols += list(range(cq + i * 64, cq + i * 64 + 64)) + list(range(cq + (i + 3) * 64, cq + (i + 3) * 64 + 64))
    cols += list(range(ak, ak + 128))
    cols += list(range(bu, bu + 512))
    cols += list(range(ck, ck + 128))
    cols += list(range(av, av + 128)) + list(range(cv, cv + 128))
    return np.array(cols)


def _perm_out():
    rows = []
    for i in range(3):
        rows += list(range(i * 64, i * 64 + 64)) + list(range((i + 3) * 64, (i + 3) * 64 + 64))
    rows += list(range(384, 640))
    for i in range(3):
        rows += list(range(640 + i * 64, 640 + i * 64 + 64)) + list(range(640 + (i + 3) * 64, 640 + (i + 3) * 64 + 64))
    return np.array(rows)


def make_in_maps(cfg, n_cores, x, c, ctx, c_ctx, norm1_g, norm2_g, w_mod, b_mod, w_in, q_norm_g, k_norm_g,
                 conv_w, conv_b, conv_ln_g, conv_ln_b, sink, w_out, w_group, b_group,
                 w_expert, b_expert, w_gate, w_up, w_down, final_g):
    f = lambda a: np.ascontiguousarray(np.asarray(a, dtype=np.float32))
    NB, S, DEPTH = cfg.NB, cfg.S, cfg.DEPTH
    x, c, ctx, c_ctx = f(x), f(c), f(ctx), f(c_ctx)
    shared = {}
    shared["n1gT"] = f(f(norm1_g).reshape(DEPTH, 8, 128).transpose(0, 2, 1))
    shared["n2gT"] = f(f(norm2_g).reshape(DEPTH, 8, 128).transpose(0, 2, 1))
    shared["w_mod"] = f(w_mod)
    shared["bmodT"] = f(f(b_mod).reshape(DEPTH, 48, 128).transpose(0, 2, 1))
    shared["b_mod"] = f(b_mod)
    shared["w_in"] = f(f(w_in)[:, :, _perm_in()])
    gq = np.tile(f(q_norm_g), (1, 2))
    gk = np.tile(f(k_norm_g), (1, 2))
    shared["gqk"] = f(np.stack([gq, gk], axis=-1))
    cw = f(conv_w)[:, :, 0, :]
    shared["convwT"] = f(cw.reshape(DEPTH, CONV_W, 2, 128).transpose(0, 3, 2, 1))
    cpar = np.stack([f(conv_b), f(conv_ln_g), f(conv_ln_b)], axis=-1)
    shared["convp"] = f(cpar.reshape(DEPTH, 2, 128, 3).transpose(0, 2, 1, 3))
    sk = f(sink)
    sb_ = np.zeros((DEPTH, 128, 3), np.float32)
    sb_[:, :64, :] = sk[:, None, 0:3]
    sb_[:, 64:, :] = sk[:, None, 3:6]
    shared["sinkb"] = sb_
    shared["w_out"] = f(f(w_out)[:, _perm_out(), :])
    shared["wr"] = f(np.concatenate([f(w_group), f(w_expert)], axis=-1))
    shared["br"] = f(np.concatenate([f(b_group), f(b_expert)], axis=-1))
    shared["w_gate"] = f(w_gate).reshape(DEPTH * NE * 128, 8 * DE)
    shared["w_up"] = f(w_up).reshape(DEPTH * NE * 128, 8 * DE)
    shared["w_down"] = f(w_down).reshape(DEPTH * NE * 128, 4 * D)
    shared["final_g"] = f(final_g)
    for k, v in host_consts(cfg).items():
        shared["k_" + k] = v
    maps = []
    for ci in range(n_cores):
        bs = slice(ci * NB, (ci + 1) * NB)
        m = dict(shared)
        m["x_in"] = f(x[bs].reshape(NB * S, D))
        m["ctx_in"] = f(ctx[bs].reshape(NB * L, D))
        cc = np.concatenate([c[bs], c_ctx[None, :]], axis=0)
        m["cT"] = f(cc.reshape(NB + 1, 8, 128).transpose(2, 1, 0))
        maps.append(m)
    return maps


_NC_CACHE = {}


def kernel(**inputs):
    x = np.asarray(inputs["x"])
    B, S, _ = x.shape
    DEPTH = np.asarray(inputs["w_in"]).shape[0]
    n_cores = 8
    NB = B // n_cores
    cfg = Cfg(NB=NB, S=S, DEPTH=DEPTH)
    key = (NB, S, DEPTH)
    if key not in _NC_CACHE:
        _NC_CACHE[key] = build(cfg)
    nc = _NC_CACHE[key]
    maps = make_in_maps(cfg, n_cores, **inputs)
    res = run_bass_kernel_spmd(nc, maps, core_ids=list(range(n_cores)))
    outs = [np.asarray(r["out"]).reshape(NB, S, D) for r in res.results]
    return np.concatenate(outs, axis=0).astype(np.float32)
```

```python
import contextlib
from contextlib import ExitStack
import numpy as np
import concourse.bass as bass
import concourse.mybir as mybir
from concourse.bass_utils import run_bass_kernel_spmd

F32 = mybir.dt.float32
BF16 = mybir.dt.bfloat16
I32 = mybir.dt.int32
AF = mybir.ActivationFunctionType
ALU = mybir.AluOpType
AX = mybir.AxisListType

SAME_ENG_SYNC = True
COMPUTE = ("pe", "act", "dve", "pool")


class Op:
    __slots__ = ("eng", "fn", "deps", "signal", "dma", "idx", "tok")

    def __init__(self, eng, fn, dma, signal):
        self.eng = eng
        self.fn = fn
        self.deps = []
        self.signal = signal
        self.dma = dma
        self.tok = None


class Sched:
    def __init__(self, nc, es, nslots=None):
        self.nc = nc
        self.es = es
        self.streams = {e: [] for e in ("pe", "act", "dve", "pool", "sp")}
        self.nslots = nslots or {"sp": 8, "act": 4, "pool": 8}
        self.last_w = {}
        self.readers = {}
        self.sb_bytes = 0

    def sb(self, name, shape, dtype):
        n = 1
        for s in shape[1:]:
            n *= s
        self.sb_bytes += n * (2 if dtype == BF16 else 4)
        return self.es.enter_context(self.nc.sbuf_tensor(name, list(shape), dtype))

    def ps(self, name, shape, dtype):
        return self.es.enter_context(self.nc.psum_tensor(name, list(shape), dtype))

    def _target(self, op):
        if op.dma or op.signal:
            return op
        st = self.streams[op.eng]
        for j in range(op.idx + 1, len(st)):
            o = st[j]
            if (not o.dma) and o.signal:
                return o
        op.signal = True
        return op

    def op(self, eng, fn, reads=(), writes=(), dma=False, signal=True):
        o = Op(eng, fn, dma, signal)
        st = self.streams[eng]
        o.idx = len(st)
        deps = []
        for k in reads:
            w = self.last_w.get(k)
            if w is not None:
                deps.append(w)
        for k in writes:
            w = self.last_w.get(k)
            if w is not None:
                deps.append(w)
            deps.extend(self.readers.get(k, ()))
        seen = set()
        for d in deps:
            if d is o:
                continue
            if (not d.dma) and (not dma) and d.eng == eng:
                if eng == "pe" or not SAME_ENG_SYNC:
                    continue
            t = self._target(d)
            if id(t) in seen:
                continue
            seen.add(id(t))
            o.deps.append(t)
        st.append(o)
        for k in writes:
            self.last_w[k] = o
            self.readers[k] = []
        for k in reads:
            if k in writes:
                continue
            lst = self.readers.setdefault(k, [])
            if not dma:
                lst[:] = [r for r in lst if r.dma or r.eng != eng]
            lst.append(o)
        return o

    def emit(self):
        nc, es = self.nc, self.es
        sems = {}
        for e in COMPUTE:
            sems[e] = es.enter_context(nc.semaphore("s_" + e))
        for q, n in self.nslots.items():
            for s in range(n):
                sems[(q, s)] = es.enter_context(nc.semaphore("d_%s%d" % (q, s)))
        for e, st in self.streams.items():
            cnt = 0
            nd = 0
            for o in st:
                if o.dma:
                    n = self.nslots[e]
                    o.tok = ((e, nd % n), 16 * (nd // n + 1))
                    nd += 1
                elif o.signal:
                    cnt += 1
                    o.tok = (e, cnt)
        engmap = {"pe": "tensor", "act": "scalar", "dve": "vector", "pool": "gpsimd", "sp": "sync"}

        def replay(ename, eng):
            known = {}
            for o in self.streams[ename]:
                waits = {}
                for d in o.deps:
                    k, v = d.tok
                    if waits.get(k, 0) < v:
                        waits[k] = v
                if o.dma:
                    k, v = o.tok
                    if v > 16 and waits.get(k, 0) < v - 16:
                        waits[k] = v - 16
                for k, v in waits.items():
                    if known.get(k, 0) >= v:
                        continue
                    known[k] = v
                    eng.wait_ge(sems[k], v)
                ins = o.fn(eng)
                if o.dma:
                    ins.then_inc(sems[o.tok[0]], 16)
                elif o.signal:
                    ins.then_inc(sems[o.tok[0]], 1)
            if ename in self.nslots:
                last = {}
                for o in self.streams[ename]:
                    if o.dma:
                        last[o.tok[0]] = o.tok[1]
                for k, v in last.items():
                    if known.get(k, 0) < v:
                        eng.wait_ge(sems[k], v)

        with nc.Block() as block:
            for ename, attr in engmap.items():
                if not self.streams[ename]:
                    continue

                def mk(ename=ename):
                    def f(eng):
                        replay(ename, eng)
                    return f
                getattr(block, attr)(mk())


D = 1024
L = 256
HD = 64
NE = 32
DE = 512
NMOD = 6
EPS = 1e-6
CONV_W = 31
GRID_W = 64
DIN = 1792
BIG = 1.0e30


class StopBuild(Exception):
    pass


class Cfg:
    def __init__(self, NB=4, S=2048, DEPTH=4, MCH=4, debug=False, stop=None):
        self.NB, self.S, self.DEPTH, self.MCH, self.debug = NB, S, DEPTH, MCH, debug
        self.stop = stop
        self.T = NB * (S + L)
        self.NT = self.T // 128
        self.CH = 128 * MCH
        self.NCH = -(-(2 * self.T) // self.CH) + NE
        self.NSLOT = self.NCH * self.CH
        self.NKB = (S + L) // 128
        self.NQB = S // 128


def host_consts(cfg):
    S = cfg.S
    c = {}
    c["ident"] = np.eye(128, dtype=np.float32)
    sw = np.zeros((128, 128), np.float32)
    for p in range(128):
        sw[p, (p + 64) % 128] = 1.0
    c["swap"] = sw
    bo = np.zeros((128, 128), np.float32)
    bo[:64, :64] = 1.0
    bo[64:, 64:] = 1.0
    c["blockones"] = bo
    RT = np.zeros((128, 128), np.float32)
    for m in range(128):
        dd = m % 64
        half = (dd % 32) // 16
        if half == 0:
            RT[m + 16, m] = -1.0
        else:
            RT[m - 16, m] = 1.0
    c["ropeRT"] = RT
    pos = np.arange(S)
    prow = (pos // GRID_W).astype(np.float32)
    pcol = (pos % GRID_W).astype(np.float32)
    inv = (10000.0 ** (-np.arange(16, dtype=np.float32) / 16)).astype(np.float32)
    cosT = np.zeros((128, S), np.float32)
    sinT = np.zeros((128, S), np.float32)
    for p in range(128):
        dd = p % 64
        seg = dd // 32
        j = dd % 16
        ang = (prow if seg == 0 else pcol) * inv[j]
        cosT[p] = np.cos(ang)
        sinT[p] = np.sin(ang)
    c["cosT"] = cosT
    c["sinT"] = sinT
    kk = np.arange(128)[:, None]
    qq = np.arange(128)[None, :]
    mlo = (qq <= kk).astype(np.float32)
    mhi = (kk <= qq).astype(np.float32)
    c["mlo"] = np.tile(mlo, (1, 3))
    c["mhi"] = np.tile(mhi, (1, 3))
    c["tri"] = (np.arange(128)[:, None] < np.arange(128)[None, :]).astype(np.float32)
    c["jpos"] = np.tile((np.arange(cfg.NCH, dtype=np.float32) * cfg.CH)[None, :], (128, 1))
    c["pidx"] = np.arange(128, dtype=np.float32).reshape(128, 1)
    return c


CONST_SHAPES = None


def build(cfg):
    NB, S, DEPTH, T, NT, NCH, CH, MCH = cfg.NB, cfg.S, cfg.DEPTH, cfg.T, cfg.NT, cfg.NCH, cfg.CH, cfg.MCH
    NBP = NB + 1
    NKB, NQB = cfg.NKB, cfg.NQB
    SL = S + L
    nc = bass.Bass("TRN2", target_bir_lowering=False)

    def din(name, shape, dt=F32):
        return nc.dram_tensor(name, list(shape), dt, kind="ExternalInput").ap()

    x_in = din("x_in", [NB * S, D])
    ctx_in = din("ctx_in", [NB * L, D])
    cT_in = din("cT", [128, 8, NBP])
    n1gT = din("n1gT", [DEPTH, 128, 8])
    n2gT = din("n2gT", [DEPTH, 128, 8])
    w_mod = din("w_mod", [DEPTH, D, NMOD * D])
    bmodT = din("bmodT", [DEPTH, 128, 48])
    b_mod = din("b_mod", [DEPTH, NMOD * D])
    w_in = din("w_in", [DEPTH, D, DIN])
    gqk = din("gqk", [DEPTH, 128, 2])
    convwT = din("convwT", [DEPTH, 128, 2, CONV_W])
    convp = din("convp", [DEPTH, 128, 2, 3])
    sinkb = din("sinkb", [DEPTH, 128, 3])
    w_out = din("w_out", [DEPTH, D, D])
    wr = din("wr", [DEPTH, D, 36])
    br = din("br", [DEPTH, 36])
    w_gate = din("w_gate", [DEPTH * NE * 128, 8 * DE])
    w_up = din("w_up", [DEPTH * NE * 128, 8 * DE])
    w_down = din("w_down", [DEPTH * NE * 128, 4 * D])
    final_g = din("final_g", [D])
    consts = host_consts(cfg)
    cin = {k: din("k_" + k, v.shape) for k, v in consts.items()}
    out = nc.dram_tensor("out", [NB * S, D], F32, kind="ExternalOutput").ap()

    def dscr(name, shape, dt):
        return nc.dram_tensor(name, list(shape), dt, kind="Internal").ap()

    xres = dscr("xres", [T, D], F32)
    h2buf = dscr("h2buf", [T, D], BF16)
    xbuf = dscr("xbuf", [cfg.NSLOT, D], BF16)
    obuf = dscr("obuf", [cfg.NSLOT, D], F32)
    modrows = dscr("modrows", [NBP, 4 * D], F32)
    dbg = {}
    if cfg.debug:
        dbg["xmid"] = nc.dram_tensor("dbg_xmid", [T, D], F32, kind="ExternalOutput").ap()
        dbg["route"] = nc.dram_tensor("dbg_route", [128, NT, 6], F32, kind="ExternalOutput").ap()
        dbg["h2"] = nc.dram_tensor("dbg_h2", [T, D], F32, kind="ExternalOutput").ap()
        dbg["bc"] = nc.dram_tensor("dbg_bc", [3, 128, D], F32, kind="ExternalOutput").ap()
        dbg["ss"] = nc.dram_tensor("dbg_ss", [NT, 128, 4], F32, kind="ExternalOutput").ap()
        dbg["rt"] = nc.dram_tensor("dbg_rt", [NT, 128, 64], F32, kind="ExternalOutput").ap()

    with ExitStack() as es:
        Sx = Sched(nc, es)
        sb, op = Sx.sb, Sx.op

        def dma(q, o, i, reads=(), writes=()):
            op(q, lambda e: e.dma_start(out=o, in_=i), reads=reads, writes=writes, dma=True)

        def mm(o, lhsT, rhs, start, stop, reads, writes, signal=True):
            op("pe", lambda e: e.matmul(o, lhsT=lhsT, rhs=rhs, start=start, stop=stop), reads=reads, writes=writes, signal=signal)

        def tr(o, i, ident, reads, writes, signal=True):
            op("pe", lambda e: e.transpose(out=o, in_=i, identity=ident), reads=reads, writes=writes, signal=signal)

        def act(o, i, func, reads, writes, scale=1.0, bias=0.0, accum_out=None):
            if accum_out is None:
                op("act", lambda e: e.activation(out=o, in_=i, func=func, bias=bias, scale=scale), reads=reads, writes=writes)
            else:
                op("act", lambda e: e.activation(out=o, in_=i, func=func, bias=bias, scale=scale, accum_out=accum_out), reads=reads, writes=writes)

        def tt(o, a, b, alu, reads, writes, eng="dve"):
            op(eng, lambda e: e.tensor_tensor(out=o, in0=a, in1=b, op=alu), reads=reads, writes=writes)

        def ts(o, a, s1, s2, op0, op1, reads, writes, eng="dve"):
            if op1 is None:
                op(eng, lambda e: e.tensor_scalar(out=o, in0=a, scalar1=s1, scalar2=None, op0=op0), reads=reads, writes=writes)
            else:
                op(eng, lambda e: e.tensor_scalar(out=o, in0=a, scalar1=s1, scalar2=s2, op0=op0, op1=op1), reads=reads, writes=writes)

        def stt(o, a, s, b, op0, op1, reads, writes, eng="dve"):
            op(eng, lambda e: e.scalar_tensor_tensor(out=o, in0=a, scalar=s, in1=b, op0=op0, op1=op1), reads=reads, writes=writes)

        def cp(o, i, reads, writes, eng="dve"):
            op(eng, lambda e: e.tensor_copy(out=o, in_=i), reads=reads, writes=writes)

        def recip(o, i, reads, writes):
            op("dve", lambda e: e.reciprocal(out=o, in_=i), reads=reads, writes=writes)

        def red(o, i, alu, reads, writes):
            op("dve", lambda e: e.tensor_reduce(out=o, in_=i, axis=AX.X, op=alu), reads=reads, writes=writes)

        def memset(o, v, writes, eng="dve"):
            op(eng, lambda e: e.memset(o, v), writes=writes)

        banks = [Sx.ps("pb%d" % i, [128, 512], F32) for i in range(8)]

        class Rot:
            def __init__(self, ids):
                self.ids, self.i = ids, 0

            def next(self):
                b = self.ids[self.i % len(self.ids)]
                self.i += 1
                return banks[b], "pb%d" % b

        rot_tr = Rot([0, 1])
        rot_mm = Rot([2, 3])
        rot_acc = Rot([4, 5])
        rot_s = Rot([6, 7, 3, 2])
        rot_d = Rot([6, 7])
        att_ctr = [0]

        def cf32(name, shape):
            t = sb("c_" + name, shape, F32)
            dma("sp", t[:], cin[name], writes=["c_" + name])
            return t

        def cbf(name, shape):
            t = sb("cb_" + name, shape, BF16)
            dma("pool", t[:], cin[name], writes=["cb_" + name])
            return t

        identf = cf32("ident", [128, 128])
        identb = cbf("ident", [128, 128])
        swapf = cf32("swap", [128, 128])
        blockones_b = cbf("blockones", [128, 128])
        ropeRT_b = cbf("ropeRT", [128, 128])
        cosT = cbf("cosT", [128, S])
        sinT = cbf("sinT", [128, S])
        mlo_b = cbf("mlo", [128, 384])
        mhi_b = cbf("mhi", [128, 384])
        trif = cf32("tri", [128, 128])
        jpos = cf32("jpos", [128, NCH])
        pidx = cf32("pidx", [128, 1])
        onesf = sb("onesf", [128, 128], F32)
        memset(onesf[:], 1.0, ["onesf"])
        onesb = sb("onesb", [128, 128], BF16)
        memset(onesb[:], 1.0, ["onesb"])
        zerosf = sb("zerosf", [128, 128], F32)
        memset(zerosf[:], 0.0, ["zerosf"])
        scT = sb("scT", [128, 8, NBP], F32)
        dma("sp", scT[:], cT_in, writes=["scT"])
        act(scT[:], scT[:], AF.Silu, ["scT"], ["scT"])

        wq = sb("wq", [128, 8, 768], BF16)
        wkvo = sb("wkvo", [128, 8, 1024], BF16)
        wrt = sb("wrt", [128, 8, 36], F32)
        brt = sb("brt", [128, 36], F32)
        modT = sb("modT", [128, 48, NBP], F32)
        G2c = sb("G2c", [128, 8, NBP], F32)
        n2T = sb("n2T", [128, 8], F32)
        dg = [sb("dg%d" % i, [128, 128], F32) for i in range(2)]
        bmT = sb("bmT", [128, 48], F32)
        n1T = sb("n1T", [128, 8], F32)
        G1 = sb("G1", [128, 8, NBP], F32)
        gqk_t = sb("gqk_t", [128, 2], F32)
        cw_t = sb("cw_t", [128, 2, CONV_W], F32)
        cpar = sb("cpar", [128, 2, 3], F32)
        diagw = sb("diagw", [128, CONV_W, 128], BF16)
        esk3 = sb("esk3", [128, 3], F32)
        esink_t = sb("esink_t", [128, 3, 128], F32)
        WMC = 256
        g1bc = sb("g1bc", [128, D], F32)
        G2bc = sb("G2bc", [128, D], F32)
        sh2bc = sb("sh2bc", [128, D], F32)
        ARN = max(8 * DE, NKB * 256, 2 * SL, 2 * (S + 30) + 2 * (L + 30))
        AR = [sb("AR%d" % i, [128, ARN], BF16) for i in range(6)]
        VA = AR[0][:, 0:NKB * 256].rearrange("p (k g c) -> p k g c", g=2, c=128)
        VC = AR[1][:, 0:NKB * 256].rearrange("p (k g c) -> p k g c", g=2, c=128)
        kA = AR[2][:, 0:2 * SL].rearrange("p (g t) -> p g t", g=2)
        kC = AR[3][:, 0:2 * SL].rearrange("p (g t) -> p g t", g=2)
        hgl = AR[4][:, 0:2 * (S + 30)].rearrange("p (c t) -> p c t", c=2)
        hglc = AR[4][:, 2 * (S + 30):2 * (S + 30) + 2 * (L + 30)].rearrange("p (c t) -> p c t", c=2)
        cvo = AR[5][:, 0:2 * SL].rearrange("p (c t) -> p c t", c=2)
        wg = [AR[0][:, 0:8 * DE], AR[3][:, 0:8 * DE]]
        wu = [AR[1][:, 0:8 * DE], AR[4][:, 0:8 * DE]]
        wd = [AR[2][:, 0:4 * D], AR[5][:, 0:4 * D]]
        wgk, wuk, wdk = ["AR0", "AR3"], ["AR1", "AR4"], ["AR2", "AR5"]
        wmv = [AR[i][:, 0:4096].bitcast(F32).rearrange("p (c n) -> p c n", c=8) for i in range(4)]
        o12s = [[AR[2 * i + k][:, 0:2048].bitcast(F32) for k in range(2)] for i in range(2)]
        o12sk = [["AR%d" % (2 * i + k) for k in range(2)] for i in range(2)]
        qA = sb("qA", [128, 3, 512], BF16)
        qC = sb("qC", [128, 4, 3, 128], BF16)
        mixc = sb("mixc", [128, 6, 512], BF16)
        xt = [sb("xt%d" % i, [128, D], F32) for i in range(2)]
        xn = sb("xn", [128, 4, D], BF16)
        hT = sb("hT", [128, 8, 512], BF16)
        ss = sb("ss", [128, 4], F32)
        t512 = [sb("t512_%d" % i, [128, 512], F32) for i in range(4)]
        b512 = [sb("b512_%d" % i, [128, 512], BF16) for i in range(3)]
        pT = [sb("pT%d" % i, [128, 512], BF16) for i in range(3)]
        o12 = [xn[:, 2 * k:2 * k + 2, :].rearrange("p a b -> p (a b)").bitcast(F32) for k in range(2)]
        o12k = [[("xn", 0), ("xn", 1)], [("xn", 2), ("xn", 3)]]
        h2T = hT[:, 0:4, :].rearrange("p a b -> p (a b)").bitcast(F32).rearrange("p (c t) -> p c t", t=128)
        h2Tk = [("hT", c) for c in range(4)]
        O1a = sb("O1a", [128, NT, NE], BF16)
        O2a = sb("O2a", [128, NT, NE], BF16)
        r12 = sb("r12", [128, NT, 2], F32)
        w12 = sb("w12", [128, NT, 2], F32)
        dst = sb("dst", [128, NT, 2], I32)
        cum = sb("cum", [128, NE], F32)
        rt = sb("rt", [128, 64], F32)
        rtb = sb("rtb", [128, 4, NE], F32)
        pst = sb("pst", [128, NE + 1], F32)
        pad_i = sb("pad_i", [128, NE], I32)
        cexp = sb("cexp", [128, NCH], F32)
        ctmp = sb("ctmp", [128, NCH], F32)
        gidx = sb("gidx", [128, NCH], I32)
        h2f = sb("h2f", [128, D], F32)
        h2fs = [h2f[:], xn[:, 0:2, :].rearrange("p a b -> p (a b)").bitcast(F32)]
        h2fsk = [["h2f"], [("xn", 0), ("xn", 1)]]
        xb = [sb("xb%d" % i, [128, D], BF16) for i in range(2)]
        xbT2 = [sb("xbT%d" % i, [128, 8, 128], BF16) for i in range(2)]
        a_sb2 = [sb("a_sb%d" % i, [128, DE], BF16) for i in range(2)]
        aT2 = [sb("aT%d" % i, [128, 4, 128], BF16) for i in range(2)]
        ob2 = [h2f, sb("ob1", [128, D], F32)]
        ob2k = ["h2f", "ob1"]

        def segs_of(b):
            return [("lat", b, b * S, S, b), ("ctx", b, NB * S + b * L, L, NB)]

        def src_rows(l, row0, n):
            if l == 0:
                if row0 < NB * S:
                    return x_in[row0:row0 + n, :]
                return ctx_in[row0 - NB * S:row0 - NB * S + n, :]
            return xres[row0:row0 + n, :]

        xti = [0]

        def load_x(l, row0):
            i = xti[0] % 2
            xti[0] += 1
            dma("sp", xt[i][:], src_rows(l, row0, 128), reads=[("xres", row0)], writes=["xt%d" % i])
            return xt[i], "xt%d" % i

        def rstd_of(xtile, xk, col, junk_ap, junk_k):
            act(junk_ap, xtile[:], AF.Square, [xk], list(junk_k) + [("ss", col)], accum_out=ss[:, col:col + 1])
            act(ss[:, col:col + 1], ss[:, col:col + 1], AF.Ln, [("ss", col)], [("ss", col)], scale=1.0 / D, bias=EPS)
            act(ss[:, col:col + 1], ss[:, col:col + 1], AF.Exp, [("ss", col)], [("ss", col)], scale=-0.5)

        def make_hT(l, row0, W, mj):
            ntl = W // 128
            for ti in range(ntl):
                xtile, xk = load_x(l, row0 + ti * 128)
                rstd_of(xtile, xk, ti, xn[:, ti, :], [("xn", ti)])
                ts(xn[:, ti, :], xtile[:], ss[:, ti:ti + 1], None, ALU.mult, None, [xk, ("ss", ti)], [("xn", ti)])
            for c in range(8):
                ps, pk = rot_tr.next()
                psb = ps[:].bitcast(BF16)
                for ti in range(ntl):
                    tr(psb[:, ti * 128:(ti + 1) * 128], xn[:, ti, c * 128:(c + 1) * 128], identb[:], [("xn", ti), "cb_ident"], [pk], signal=(ti == ntl - 1))
                act(hT[:, c, 0:W], psb[:, 0:W], AF.Identity, [pk, "G1", "modT"], [("hT", c)], scale=G1[:, c, mj:mj + 1], bias=modT[:, c, mj:mj + 1])

        hTk = [("hT", c) for c in range(8)]

        def proj(wt, wk, c0, W):
            ps, pk = rot_mm.next()
            for c in range(8):
                mm(ps[:, 0:W], wt[:, c, c0:c0 + 128], hT[:, c, 0:W], c == 0, c == 7, [wk] + hTk, [pk], signal=(c == 7))
            return ps, pk

        def rope_store(src, srck, W, tcols, dst_fn):
            ps, pk = rot_tr.next()
            mm(ps[:, 0:W], ropeRT_b[:], src[:, 0:W], True, True, ["cb_ropeRT", srck], [pk])
            tt(t512[0][:, 0:W], src[:, 0:W], cosT[:, tcols], ALU.mult, [srck, "cb_cosT"], ["t512_0"])
            tt(t512[1][:, 0:W], ps[:, 0:W], sinT[:, tcols], ALU.mult, [pk, "cb_sinT"], ["t512_1"])
            dst_fn(t512[0], t512[1])

        def qk_norm(ps, pk, gcol, W):
            act(b512[0][:, 0:W], ps[:, 0:W], AF.Square, [pk], ["b512_0"])
            p2, p2k = rot_tr.next()
            mm(p2[:, 0:W], blockones_b[:], b512[0][:, 0:W], True, True, ["cb_blockones", "b512_0"], [p2k])
            act(t512[2][:, 0:W], p2[:, 0:W], AF.Ln, [p2k], ["t512_2"], scale=1.0 / HD, bias=EPS)
            act(t512[2][:, 0:W], t512[2][:, 0:W], AF.Exp, ["t512_2"], ["t512_2"], scale=-0.5)
            stt(b512[1][:, 0:W], ps[:, 0:W], gqk_t[:, gcol:gcol + 1], t512[2][:, 0:W], ALU.mult, ALU.mult, [pk, "gqk_t", "t512_2"], ["b512_1"])
            return b512[1], "b512_1"

        def attention(q_ap, qk_, W, keyblocks, kT, kTk, V, Vk, dst_top, dst_bot, sink):
            nk = len(keyblocks)
            steps = [(g, idx) for g in range(2) for idx in range(nk)]
            accs = [rot_acc.next(), rot_acc.next()]
            LA = 3

            def qk_mm(sidx):
                g, idx = steps[sidx]
                kb = keyblocks[idx][0]
                ps, pk = rot_s.next()
                mm(ps[:, 0:W], kT[:, g, kb * 128:(kb + 1) * 128], q_ap, True, True, [kTk, qk_], [pk])
                return ps, pk

            issued = [qk_mm(i) for i in range(min(LA, len(steps)))]
            for sidx, (g, idx) in enumerate(steps):
                kb, mask, mk_ = keyblocks[idx]
                ps, pk = issued[sidx]
                acc, ak = accs[g]
                pi = att_ctr[0] % len(pT)
                att_ctr[0] += 1
                act(pT[pi][:, 0:W], ps[:, 0:W], AF.Exp, [pk], ["pT%d" % pi], scale=HD ** -0.5)
                if mask is not None:
                    tt(pT[pi][:, 0:W], pT[pi][:, 0:W], mask[:, 0:W], ALU.mult, ["pT%d" % pi, mk_], ["pT%d" % pi])
                mm(acc[:, 0:W], V[:, kb, g, :], pT[pi][:, 0:W], idx == 0, idx == nk - 1, [Vk, "pT%d" % pi], [ak], signal=(idx == nk - 1))
                if sidx + LA < len(steps):
                    issued.append(qk_mm(sidx + LA))
            (a0, a0k), (a1, a1k) = accs
            R = t512[2]
            act(R[64:128, 0:W], a0[64:128, 0:W], AF.Identity, [a0k], ["t512_2"])
            act(R[0:64, 0:W], a1[0:64, 0:W], AF.Identity, [a1k], ["t512_2"])
            ps, pk = rot_s.next()
            mm(ps[:, 0:W], swapf[:], R[:, 0:W], True, True, ["c_swap", "t512_2"], [pk])
            if sink:
                tt(t512[3][:, 0:W], ps[:, 0:W], esink_t[:].rearrange("p a b -> p (a b)")[:, 0:W], ALU.add, [pk, "esink_t"], ["t512_3"])
                act(t512[3][:, 0:W], t512[3][:, 0:W], AF.Ln, ["t512_3"], ["t512_3"])
            else:
                act(t512[3][:, 0:W], ps[:, 0:W], AF.Ln, [pk], ["t512_3"])
            act(t512[3][:, 0:W], t512[3][:, 0:W], AF.Exp, ["t512_3"], ["t512_3"], scale=-1.0)
            dst_top(a0, a0k, t512[3])
            dst_bot(a1, a1k, t512[3])

        dgi = [0]

        def build_bc(dst_t, dk, src_t, srck, c_off, j):
            for half in range(2):
                ps, pk = rot_mm.next()
                for c4 in range(4):
                    c = half * 4 + c4
                    di = dgi[0] % 2
                    dgi[0] += 1
                    ts(dg[di][:], identf[:], src_t[:, c_off + c, j:j + 1], None, ALU.mult, None, ["c_ident", srck], ["dg%d" % di])
                    mm(ps[:, c4 * 128:(c4 + 1) * 128], onesf[:], dg[di][:], True, True, ["onesf", "dg%d" % di], [pk])
                act(dst_t[:, half * 512:(half + 1) * 512], ps[:, :], AF.Identity, [pk], [dk])

        def stop_at(name):
            if cfg.stop == name:
                raise StopBuild()

        try:
            for l in range(DEPTH):
                last = (l == DEPTH - 1)
                w_in_v = w_in[l].rearrange("(c p) n -> p c n", p=128)
                dma("pool", wq[:], w_in_v[:, :, 0:768], writes=["wq"])
                dma("sp", wrt[:], wr[l].rearrange("(c p) n -> p c n", p=128), writes=["wrt"])
                dma("sp", brt[:], br[l].partition_broadcast(128), writes=["brt"])
                dma("sp", bmT[:], bmodT[l], writes=["bmT"])
                dma("sp", n1T[:], n1gT[l], writes=["n1T"])
                dma("sp", n2T[:], n2gT[l], writes=["n2T"])
                dma("sp", gqk_t[:], gqk[l], writes=["gqk_t"])
                dma("sp", cw_t[:], convwT[l], writes=["cw_t"])
                dma("sp", cpar[:], convp[l], writes=["cpar"])
                dma("sp", esk3[:], sinkb[l], writes=["esk3"])
                act(esk3[:], esk3[:], AF.Exp, ["esk3"], ["esk3"])
                for hh in range(3):
                    ts(esink_t[:, hh, :], zerosf[:, :], esk3[:, hh:hh + 1], None, ALU.add, None, ["zerosf", "esk3"], ["esink_t"])
                stop_at("params")
                stop_at("arena")
                wmod_v = w_mod[l].rearrange("(c p) n -> p c n", p=128)
                npieces = (6 * D) // WMC
                for piece in range(npieces):
                    wt_, wk = wmv[piece % 4], "AR%d" % (piece % 4)
                    dma("sp", wt_, wmod_v[:, :, piece * WMC:(piece + 1) * WMC], writes=[wk])
                    for f in range(WMC // 128):
                        fc = piece * (WMC // 128) + f
                        ps, pk = rot_mm.next()
                        for c in range(8):
                            mm(ps[:, 0:NBP], wt_[:, c, f * 128:(f + 1) * 128], scT[:, c, :], c == 0, c == 7, [wk, "scT"], [pk], signal=(c == 7))
                        act(modT[:, fc, :], ps[:, 0:NBP], AF.Identity, [pk, "bmT"], ["modT"], bias=bmT[:, fc:fc + 1])
                for c in range(8):
                    ts(G1[:, c, :], modT[:, 8 + c, :], 1.0, n1T[:, c:c + 1], ALU.add, ALU.mult, ["modT", "n1T"], ["G1"])
                    ts(G2c[:, c, :], modT[:, 32 + c, :], 1.0, n2T[:, c:c + 1], ALU.add, ALU.mult, ["modT", "n2T"], ["G2c"])

                memset(AR[0][:], 1.0, ["AR0"])
                memset(AR[1][:], 1.0, ["AR1"])
                memset(AR[2][:], 0.0, ["AR2"])
                memset(AR[3][:], 0.0, ["AR3"])
                memset(AR[4][:], 0.0, ["AR4"], eng="pool")
                stop_at("mod")
                memset(cum[:], 0.0, ["cum"])
                tile_ctr = [0]
                moe_tiles = []
                for b in range(NB):
                    dma("pool", wkvo[:], w_in_v[:, :, 768:DIN], reads=[], writes=["wkvo"])
                    for (kind, _, row0, ntok, mj) in segs_of(b):
                        is_lat = kind == "lat"
                        col_base = 0 if is_lat else S
                        for g0 in range(0, ntok, 512):
                            W = min(512, ntok - g0)
                            ntl = W // 128
                            make_hT(l, row0 + g0, W, mj)
                            cols = slice(col_base + g0, col_base + g0 + W)
                            tcols = slice(g0, g0 + W)
                            nb0 = (col_base + g0) // 128
                            stop_at("hT")
                            ps, pk = proj(wkvo, "wkvo", 0, W)
                            kn, knk = qk_norm(ps, pk, 1, W)
                            if is_lat:
                                def kst(a, bb, cols=cols, W=W):
                                    tt(kA[0:64, 0, cols], a[0:64, 0:W], bb[0:64, 0:W], ALU.add, ["t512_0", "t512_1"], ["AR2"])
                                    tt(kA[64:128, 1, cols], a[64:128, 0:W], bb[64:128, 0:W], ALU.add, ["t512_0", "t512_1"], ["AR2"])
                                rope_store(kn, knk, W, tcols, kst)
                            else:
                                cp(kA[0:64, 0, cols], kn[0:64, 0:W], [knk], ["AR2"])
                                cp(kA[64:128, 1, cols], kn[64:128, 0:W], [knk], ["AR2"])
                            stop_at("ak")
                            hg = hgl if is_lat else hglc
                            for cc in range(2):
                                psg, pgk = proj(wkvo, "wkvo", (3 + cc) * 128, W)
                                act(t512[3][:, 0:W], psg[:, 0:W], AF.Sigmoid, [pgk], ["t512_3"])
                                psa, pak = proj(wkvo, "wkvo", (1 + cc) * 128, W)
                                tt(hg[:, cc, 15 + g0:15 + g0 + W], psa[:, 0:W], t512[3][:, 0:W], ALU.mult, [pak, "t512_3"], ["AR4"])
                            stop_at("glu")
                            ps, pk = proj(wkvo, "wkvo", 5 * 128, W)
                            if is_lat:
                                act(b512[2][:, 0:W], ps[:, 0:W], AF.Identity, [pk], ["b512_2"])

                                def kcst(a, bb, cols=cols, W=W):
                                    tt(kC[0:64, 0, cols], a[0:64, 0:W], bb[0:64, 0:W], ALU.add, ["t512_0", "t512_1"], ["AR3"])
                                    tt(kC[64:128, 1, cols], a[64:128, 0:W], bb[64:128, 0:W], ALU.add, ["t512_0", "t512_1"], ["AR3"])
                                rope_store(b512[2], "b512_2", W, tcols, kcst)
                            else:
                                act(kC[0:64, 0, cols], ps[0:64, 0:W], AF.Identity, [pk], ["AR3"])
                                act(kC[64:128, 1, cols], ps[64:128, 0:W], AF.Identity, [pk], ["AR3"])
                            stop_at("ck")
                            for ti in range(ntl):
                                kb = nb0 + ti
                                ps, pk = rot_mm.next()
                                for c in range(8):
                                    mm(ps[:, 0:256], hT[:, c, ti * 128:(ti + 1) * 128], wkvo[:, c, 768:1024], c == 0, c == 7, ["wkvo"] + hTk, [pk], signal=(c == 7))
                                cp(VA[:, kb, 0, 0:64], ps[:, 0:64], [pk], ["AR0"])
                                cp(VA[:, kb, 1, 64:128], ps[:, 64:128], [pk], ["AR0"])
                                cp(VC[:, kb, 0, 0:64], ps[:, 128:192], [pk], ["AR1"])
                                cp(VC[:, kb, 1, 64:128], ps[:, 192:256], [pk], ["AR1"])
                            stop_at("v1")
                    stop_at("pass1")
                    dma("pool", wkvo[:], w_out[l].rearrange("(c p) n -> p c n", p=128), writes=["wkvo"])
                    conv_segs = [(hgl, S, 0)] + ([] if last else [(hglc, L, S)])
                    for cc in range(2):
                        for j in range(CONV_W):
                            ts(diagw[:, j, :], identf[:, :], cw_t[:, cc, j:j + 1], None, ALU.mult, None, ["c_ident", "cw_t"], ["diagw"])
                        for (hg, n_tok, cb) in conv_segs:
                            for g0 in range(0, n_tok, 512):
                                W = min(512, n_tok - g0)
                                ps, pk = rot_mm.next()
                                for j in range(CONV_W):
                                    mm(ps[:, 0:W], diagw[:, j, :], hg[:, cc, g0 + j:g0 + j + W], j == 0, j == CONV_W - 1, ["diagw", "AR4"], [pk], signal=(j == CONV_W - 1))
                                act(cvo[:, cc, cb + g0:cb + g0 + W], ps[:, 0:W], AF.Identity, [pk, "cpar"], ["AR5"], bias=cpar[:, cc, 0:1])
                    for (hg, n_tok, cb) in conv_segs:
                        for g0 in range(0, n_tok, 512):
                            W = min(512, n_tok - g0)
                            cs = slice(cb + g0, cb + g0 + W)
                            p1, p1k = rot_tr.next()
                            for cc in range(2):
                                mm(p1[:, 0:W], onesb[:], cvo[:, cc, cs], cc == 0, cc == 1, ["onesb", "AR5"], [p1k], signal=(cc == 1))
                            act(t512[0][:, 0:W], p1[:, 0:W], AF.Identity, [p1k], ["t512_0"], scale=1.0 / 256)
                            p2, p2k = rot_tr.next()
                            for cc in range(2):
                                act(b512[cc][:, 0:W], cvo[:, cc, cs], AF.Square, ["AR5"], ["b512_%d" % cc])
                                mm(p2[:, 0:W], onesb[:], b512[cc][:, 0:W], cc == 0, cc == 1, ["onesb", "b512_%d" % cc], [p2k], signal=(cc == 1))
                            tt(t512[1][:, 0:W], t512[0][:, 0:W], t512[0][:, 0:W], ALU.mult, ["t512_0"], ["t512_1"])
                            stt(t512[2][:, 0:W], p2[:, 0:W], 1.0 / 256, t512[1][:, 0:W], ALU.mult, ALU.subtract, [p2k, "t512_1"], ["t512_2"])
                            act(t512[2][:, 0:W], t512[2][:, 0:W], AF.Ln, ["t512_2"], ["t512_2"], bias=EPS)
                            act(t512[2][:, 0:W], t512[2][:, 0:W], AF.Exp, ["t512_2"], ["t512_2"], scale=-0.5)
                            for cc in range(2):
                                tt(t512[3][:, 0:W], cvo[:, cc, cs], t512[0][:, 0:W], ALU.subtract, ["AR5", "t512_0"], ["t512_3"])
                                tt(t512[3][:, 0:W], t512[3][:, 0:W], t512[2][:, 0:W], ALU.mult, ["t512_3", "t512_2"], ["t512_3"])
                                act(cvo[:, cc, cs], t512[3][:, 0:W], AF.Silu, ["t512_3", "cpar"], ["AR5"], scale=cpar[:, cc, 1:2], bias=cpar[:, cc, 2:3])

                    stop_at("conv")
                    ctxkeys = [(NQB, None, None), (NQB + 1, None, None)]
                    allkeys = [(kb, None, None) for kb in range(NKB)]
                    for (kind, _, row0, ntok, mj) in segs_of(b):
                        is_lat = kind == "lat"
                        if last and not is_lat:
                            continue
                        col_base = 0 if is_lat else S
                        build_bc(g1bc, "g1bc", modT, "modT", 16, mj)
                        build_bc(sh2bc, "sh2bc", modT, "modT", 24, mj)
                        build_bc(G2bc, "G2bc", G2c, "G2c", 0, mj)
                        if cfg.debug and l == 0 and b == 0 and is_lat:
                            dma("sp", dbg["bc"][0], g1bc[:], reads=["g1bc"])
                            dma("sp", dbg["bc"][1], sh2bc[:], reads=["sh2bc"])
                            dma("sp", dbg["bc"][2], G2bc[:], reads=["G2bc"])
                        for g0 in range(0, ntok, 512):
                            W = min(512, ntok - g0)
                            ntl = W // 128
                            make_hT(l, row0 + g0, W, mj)
                            tcols = slice(g0, g0 + W)
                            for i in range(3):
                                ps, pk = proj(wq, "wq", i * 128, W)
                                qn, qnk = qk_norm(ps, pk, 0, W)
                                if is_lat:
                                    rope_store(qn, qnk, W, tcols, lambda a, bb, i=i, W=W: tt(qA[:, i, 0:W], a[:, 0:W], bb[:, 0:W], ALU.add, ["t512_0", "t512_1"], ["qA"]))
                                else:
                                    cp(qA[:, i, 0:W], qn[:, 0:W], [qnk], ["qA"])
                            for i in range(3):
                                ps, pk = proj(wq, "wq", (3 + i) * 128, W)
                                if is_lat:
                                    act(b512[2][:, 0:W], ps[:, 0:W], AF.Identity, [pk], ["b512_2"])
                                    rope_store(b512[2], "b512_2", W, tcols, lambda a, bb, i=i, W=W, ntl=ntl: tt(
                                        qC[:, 0:ntl, i, :], a[:, 0:W].rearrange("p (n q) -> p n q", q=128), bb[:, 0:W].rearrange("p (n q) -> p n q", q=128),
                                        ALU.add, ["t512_0", "t512_1"], ["qC"]))
                                else:
                                    act(qC[:, 0:ntl, i, :], ps[:, 0:W].rearrange("p (n q) -> p n q", q=128), AF.Identity, [pk], ["qC"])
                            keysA = allkeys if is_lat else ctxkeys
                            for i in range(3):
                                attention(qA[:, i, 0:W], "qA", W, keysA, kA, "AR2", VA, "AR0",
                                          lambda a, ak, rc, i=i, W=W: tt(mixc[0:64, i, 0:W], a[0:64, 0:W], rc[0:64, 0:W], ALU.mult, [ak, "t512_3"], ["mixc"]),
                                          lambda a, ak, rc, i=i, W=W: tt(mixc[64:128, i, 0:W], a[64:128, 0:W], rc[64:128, 0:W], ALU.mult, [ak, "t512_3"], ["mixc"]),
                                          False)
                            def attn_c(bi, g0=g0, is_lat=is_lat):
                                if is_lat:
                                    n = g0 // 128 + bi
                                    kbs = []
                                    if n > 0:
                                        kbs.append((n - 1, mlo_b, "cb_mlo"))
                                    kbs.append((n, None, None))
                                    if n < NQB - 1:
                                        kbs.append((n + 1, mhi_b, "cb_mhi"))
                                    kbs += ctxkeys
                                else:
                                    kbs = ctxkeys
                                c0 = bi * 128
                                attention(qC[:, bi, :, :].rearrange("p a b -> p (a b)"), "qC", 384, kbs, kC, "AR3", VC, "AR1",
                                          lambda a, ak, rc, c0=c0: tt(mixc[0:64, 3:6, c0:c0 + 128], a[0:64, 0:384].rearrange("p (a b) -> p a b", b=128),
                                                                      rc[0:64, 0:384].rearrange("p (a b) -> p a b", b=128), ALU.mult, [ak, "t512_3"], [("mixcC", c0)]),
                                          lambda a, ak, rc, c0=c0: tt(mixc[64:128, 3:6, c0:c0 + 128], a[64:128, 0:384].rearrange("p (a b) -> p a b", b=128),
                                                                      rc[64:128, 0:384].rearrange("p (a b) -> p a b", b=128), ALU.mult, [ak, "t512_3"], [("mixcC", c0)]),
                                          True)
                            pre = {0: load_x(l, row0 + g0)}

                            def tile_gen(tl, pre=pre, row0=row0, g0=g0, ntl=ntl, col_base=col_base, mj=mj):
                                r0 = row0 + g0 + tl * 128
                                cg = col_base + g0 + tl * 128
                                xtile, xk = pre[tl]
                                if tl + 1 < ntl:
                                    pre[tl + 1] = load_x(l, r0 + 128)
                                hb, hbk = h2fs[tl % 2], h2fsk[tl % 2]
                                sc_ = tl % 4
                                attn_c(tl)
                                ck_ = ("mixcC", tl * 128)
                                lhs = [(mixc[:, 0, tl * 128:(tl + 1) * 128], "mixc"), (mixc[:, 1, tl * 128:(tl + 1) * 128], "mixc"), (mixc[:, 2, tl * 128:(tl + 1) * 128], "mixc"),
                                       (cvo[:, 0, cg:cg + 128], "AR5"), (cvo[:, 1, cg:cg + 128], "AR5"),
                                       (mixc[:, 3, tl * 128:(tl + 1) * 128], ck_), (mixc[:, 4, tl * 128:(tl + 1) * 128], ck_), (mixc[:, 5, tl * 128:(tl + 1) * 128], ck_)]
                                for half in range(2):
                                    hs = slice(half * 512, (half + 1) * 512)
                                    ps, pk = rot_mm.next()
                                    for c in range(8):
                                        mm(ps[:, :], lhs[c][0], wkvo[:, c, hs], c == 0, c == 7, [lhs[c][1], "wkvo"], [pk], signal=(c == 7))
                                    tt(t512[half][:, :], ps[:, :], g1bc[:, hs], ALU.mult, [pk, "g1bc"], ["t512_%d" % half])
                                    tt(xtile[:, hs], xtile[:, hs], t512[half][:, :], ALU.add, [xk, "t512_%d" % half], [xk])
                                dma("sp", xres[r0:r0 + 128, :], xtile[:], reads=[xk], writes=[("xres", r0)])
                                if cfg.debug and l == 0:
                                    dma("sp", dbg["xmid"][r0:r0 + 128, :], xtile[:], reads=[xk])
                                rstd_of(xtile, xk, sc_, hb, hbk)
                                stt(hb, xtile[:], ss[:, sc_:sc_ + 1], G2bc[:], ALU.mult, ALU.mult, [xk, ("ss", sc_), "G2bc"], hbk)
                                tt(hb, hb, sh2bc[:], ALU.add, hbk + ["sh2bc"], hbk)
                                yield
                                ti = tile_ctr[0]
                                tile_ctr[0] += 1
                                moe_tiles.append((r0, mj))
                                act(xb[0][:], hb, AF.Identity, hbk, ["xb0"])
                                if cfg.debug and l == 0:
                                    dma("sp", dbg["h2"][r0:r0 + 128, :], hb, reads=hbk)
                                    dma("sp", dbg["ss"][ti], ss[:, :], reads=[("ss", sc_)])
                                dma("sp", h2buf[r0:r0 + 128, :], xb[0][:], reads=["xb0"], writes=[("h2buf", r0)])
                                for hf in range(2):
                                    ps, pk = rot_tr.next()
                                    for c4 in range(4):
                                        c = hf * 4 + c4
                                        tr(ps[:, c4 * 128:(c4 + 1) * 128], hb[:, c * 128:(c + 1) * 128], identf[:], hbk + ["c_ident"], [pk], signal=(c4 == 3))
                                    act(h2T[:, hf * 4:hf * 4 + 4, :], ps[:, :].rearrange("p (a b) -> p a b", b=128), AF.Identity, [pk], h2Tk)
                                ps, pk = rot_mm.next()
                                for c in range(8):
                                    mm(ps[:, 0:36], h2T[:, c, :], wrt[:, c, :], c == 0, c == 7, h2Tk + ["wrt"], [pk], signal=(c == 7))
                                RT_ = "rt"
                                tt(rt[:, 0:36], ps[:, 0:36], brt[:, :], ALU.add, [pk, "brt"], [RT_])
                                red(rt[:, 36:37], rt[:, 0:4], ALU.max, [RT_], [RT_])
                                ts(rt[:, 48:49], rt[:, 36:37], -1.0, None, ALU.mult, None, [RT_], [RT_])
                                act(rt[:, 40:44], rt[:, 0:4], AF.Exp, [RT_], [RT_], bias=rt[:, 48:49], accum_out=rt[:, 37:38])
                                recip(rt[:, 37:38], rt[:, 37:38], [RT_], [RT_])
                                ts(rt[:, 40:44], rt[:, 0:4], rt[:, 36:37], None, ALU.is_equal, None, [RT_], [RT_])
                                ts(rt[:, 44:48], rt[:, 40:44], -1.0, BIG, ALU.add, ALU.mult, [RT_], [RT_])
                                for g in range(4):
                                    ts(rtb[:, 0, g * 8:(g + 1) * 8], rt[:, 4 + g * 8:12 + g * 8], rt[:, 44 + g:45 + g], None, ALU.add, None, [RT_], ["rtb"])
                                red(rt[:, 38:39], rtb[:, 0, :], ALU.max, ["rtb"], [RT_])
                                ts(O1a[:, ti, :], rtb[:, 0, :], rt[:, 38:39], None, ALU.is_equal, None, ["rtb", RT_], [("O1", ti)])
                                stt(rtb[:, 1, :], O1a[:, ti, :], -BIG, rtb[:, 0, :], ALU.mult, ALU.add, [("O1", ti), "rtb"], ["rtb"])
                                red(rt[:, 39:40], rtb[:, 1, :], ALU.max, ["rtb"], [RT_])
                                ts(O2a[:, ti, :], rtb[:, 1, :], rt[:, 39:40], None, ALU.is_equal, None, ["rtb", RT_], [("O2", ti)])
                                tt(rt[:, 48:49], rt[:, 39:40], rt[:, 38:39], ALU.subtract, [RT_], [RT_])
                                act(rt[:, 48:49], rt[:, 48:49], AF.Exp, [RT_], [RT_])
                                ts(rt[:, 48:49], rt[:, 48:49], 1.0, None, ALU.add, None, [RT_], [RT_])
                                recip(rt[:, 48:49], rt[:, 48:49], [RT_], [RT_])
                                tt(w12[:, ti, 0:1], rt[:, 48:49], rt[:, 37:38], ALU.mult, [RT_], [("w12", ti)])
                                tt(w12[:, ti, 1:2], rt[:, 37:38], w12[:, ti, 0:1], ALU.subtract, [RT_, ("w12", ti)], [("w12", ti)])
                                tt(rtb[:, 2, :], O1a[:, ti, :], O2a[:, ti, :], ALU.add, [("O1", ti), ("O2", ti)], ["rtb"])
                                ps, pk = rot_mm.next()
                                mm(ps[:, 0:NE], trif[:], rtb[:, 2, :], True, True, ["c_tri", "rtb"], [pk])
                                tt(rtb[:, 3, :], ps[:, 0:NE], cum[:], ALU.add, [pk, "cum"], ["rtb"])
                                ps2, pk2 = rot_mm.next()
                                mm(ps2[:, 0:NE], onesf[:], rtb[:, 2, :], True, True, ["onesf", "rtb"], [pk2])
                                tt(cum[:], cum[:], ps2[:, 0:NE], ALU.add, ["cum", pk2], ["cum"])
                                tt(rtb[:, 0, :], O1a[:, ti, :], rtb[:, 3, :], ALU.mult, [("O1", ti), "rtb"], ["rtb"])
                                red(r12[:, ti, 0:1], rtb[:, 0, :], ALU.add, ["rtb"], [("r12", ti)])
                                tt(rtb[:, 1, :], O2a[:, ti, :], rtb[:, 3, :], ALU.mult, [("O2", ti), "rtb"], ["rtb"])
                                red(r12[:, ti, 1:2], rtb[:, 1, :], ALU.add, ["rtb"], [("r12", ti)])
                                if cfg.debug and l == 0:
                                    dma("sp", dbg["rt"][ti], rt[:, :], reads=["rt"])

                            gens = [tile_gen(tl) for tl in range(ntl)]
                            next(gens[0])
                            for tl in range(ntl):
                                if tl + 1 < ntl:
                                    next(gens[tl + 1])
                                for _ in gens[tl]:
                                    pass

                stop_at("pass2")
                nmt = tile_ctr[0]
                ts(rtb[:, 0, :], cum[:], float(CH - 1), None, ALU.add, None, ["cum"], ["rtb"])
                cp(pad_i[:], rtb[:, 0, :], ["rtb"], ["pad_i"])
                sh = int(np.log2(CH))
                ts(pad_i[:], pad_i[:], sh, None, ALU.arith_shift_right, None, ["pad_i"], ["pad_i"])
                ts(pad_i[:], pad_i[:], sh, None, ALU.logical_shift_left, None, ["pad_i"], ["pad_i"])
                cp(rtb[:, 1, :], pad_i[:], ["pad_i"], ["rtb"])
                memset(pst[:, 0:1], 0.0, ["pst"])
                for e in range(NE):
                    tt(pst[:, e + 1:e + 2], pst[:, e:e + 1], rtb[:, 1, e:e + 1], ALU.add, ["pst", "rtb"], ["pst"])
                memset(cexp[:], 0.0, ["cexp"])
                for e in range(NE):
                    ts(ctmp[:], jpos[:], pst[:, e + 1:e + 2], None, ALU.is_ge, None, ["c_jpos", "pst"], ["ctmp"])
                    tt(cexp[:], cexp[:], ctmp[:], ALU.add, ["cexp", "ctmp"], ["cexp"])
                ts(cexp[:], cexp[:], float(NE - 1), float(l * NE), ALU.min, ALU.add, ["cexp"], ["cexp"])
                ts(ctmp[:], cexp[:], 128.0, pidx[:, 0:1], ALU.mult, ALU.add, ["cexp", "c_pidx"], ["ctmp"])
                cp(gidx[:], ctmp[:], ["ctmp"], ["gidx"])
                for ti in range(nmt):
                    for k, Oa, Ok in ((0, O1a, "O1"), (1, O2a, "O2")):
                        tt(rtb[:, k, :], Oa[:, ti, :], pst[:, 0:NE], ALU.mult, [(Ok, ti), "pst"], ["rtb"])
                        red(rt[:, k:k + 1], rtb[:, k, :], ALU.add, ["rtb"], ["rt"])
                    tt(rt[:, 2:4], rt[:, 0:2], r12[:, ti, :], ALU.add, ["rt", ("r12", ti)], ["rt"])
                    cp(dst[:, ti, :], rt[:, 2:4], ["rt"], [("dst", ti)])
                if cfg.debug and l == 0:
                    dbr = sb("dbr", [128, NT, 6], F32)
                    memset(dbr[:], 0.0, ["dbr"])
                    for ti in range(nmt):
                        cp(dbr[:, ti, 0:2], dst[:, ti, :], [("dst", ti)], ["dbr"])
                        cp(dbr[:, ti, 2:4], w12[:, ti, :], [("w12", ti)], ["dbr"])
                        cp(dbr[:, ti, 4:6], r12[:, ti, :], [("r12", ti)], ["dbr"])
                    dma("sp", dbg["route"], dbr[:], reads=["dbr"])
                stop_at("disp")
                xbw_keys = [("xbw", n) for n in range(2 * nmt)]
                for ti, (r0, mj) in enumerate(moe_tiles):
                    xbt, xbk = xb[ti % 2], "xb%d" % (ti % 2)
                    dma("sp", xbt[:], h2buf[r0:r0 + 128, :], reads=[("h2buf", r0)], writes=[xbk])
                    for k in range(2):
                        op("pool", lambda e, xbt=xbt, ti=ti, k=k: e.indirect_dma_start(
                            out=xbuf[:, :], out_offset=bass.IndirectOffsetOnAxis(ap=dst[:, ti, k:k + 1], axis=0), in_=xbt[:, :], in_offset=None),
                            reads=[xbk, ("dst", ti)], writes=[("xbw", 2 * ti + k)], dma=True)

                stop_at("scatter")
                nch_used = -(-(2 * nmt * 128) // CH) + NE
                subs = [(j, m) for j in range(nch_used) for m in range(MCH)]

                def stage_a(si):
                    j, m = subs[si]
                    wi = j % 2
                    if m == 0:
                        for (wt_, wname, src) in ((wg[wi], wgk[wi], w_gate), (wu[wi], wuk[wi], w_up), (wd[wi], wdk[wi], w_down)):
                            op("pool", lambda e, wt_=wt_, src=src, j=j: e.indirect_dma_start(
                                out=wt_, out_offset=None, in_=src[:, :], in_offset=bass.IndirectOffsetOnAxis(ap=gidx[:, j:j + 1], axis=0)),
                                reads=["gidx"], writes=[wname], dma=True)
                    s0 = j * CH + m * 128
                    xi = si % 2
                    xbt, xbk = xb[xi], "xb%d" % xi
                    xbT, xbTk = xbT2[xi], "xbT%d" % xi
                    xv = xbt[:].rearrange("s (p c) -> s c p", c=8)
                    for hf in range(2):
                        ps, pk = rot_tr.next()
                        psb = ps[:].bitcast(BF16)
                        for c4 in range(4):
                            c = hf * 4 + c4
                            tr(psb[:, c4 * 128:(c4 + 1) * 128], xv[:, c, :], identb[:], [xbk, "cb_ident"], [pk], signal=(c4 == 3))
                        act(xbT[:, hf * 4:hf * 4 + 4, :], psb[:, 0:512].rearrange("p (a b) -> p a b", b=128), AF.Identity, [pk], [xbTk])

                def stage_b(si):
                    j, m = subs[si]
                    wi = j % 2
                    xi = si % 2
                    xbT, xbTk = xbT2[xi], "xbT%d" % xi
                    psg, pgk = rot_acc.next()
                    for c in range(8):
                        mm(psg[:, :], xbT[:, c, :], wg[wi][:, c * DE:(c + 1) * DE], c == 0, c == 7, [xbTk, wgk[wi]], [pgk], signal=(c == 7))
                    psu, puk = rot_mm.next()
                    for c in range(8):
                        mm(psu[:, :], xbT[:, c, :], wu[wi][:, c * DE:(c + 1) * DE], c == 0, c == 7, [xbTk, wuk[wi]], [puk], signal=(c == 7))
                    return (psg, pgk, psu, puk)

                def stage_c(si, st):
                    j, m = subs[si]
                    wi = j % 2
                    psg, pgk, psu, puk = st
                    s0 = j * CH + m * 128
                    xi = si % 2
                    a_sb, a_sbk = a_sb2[xi], "a_sb%d" % xi
                    aT, aTk = aT2[xi], "aT%d" % xi
                    ob, obk = ob2[xi], ob2k[xi]
                    tsl, tslk = t512[xi], "t512_%d" % xi
                    act(tsl[:, :], psg[:, :], AF.Silu, [pgk], [tslk])
                    tt(a_sb[:, :], tsl[:, :], psu[:, :], ALU.mult, [tslk, puk], [a_sbk])
                    av = a_sb[:].rearrange("s (p c) -> s c p", c=4)
                    ps, pk = rot_tr.next()
                    psb = ps[:].bitcast(BF16)
                    for c in range(4):
                        tr(psb[:, c * 128:(c + 1) * 128], av[:, c, :], identb[:], [a_sbk, "cb_ident"], [pk], signal=(c == 3))
                    cp(aT[:], psb[:, 0:512].rearrange("p (a b) -> p a b", b=128), [pk], [aTk])
                    for half in range(2):
                        ps, pk = rot_d.next()
                        for c in range(4):
                            mm(ps[:, :], aT[:, c, :], wd[wi][:, c * D + half * 512:c * D + half * 512 + 512], c == 0, c == 3, [aTk, wdk[wi]], [pk], signal=(c == 3))
                        if half == 0:
                            act(ob[:, 0:512], ps[:, :], AF.Identity, [pk], [obk])
                        else:
                            cp(ob[:, 512:1024], ps[:, :], [pk], [obk])
                    dma("sp", obuf[s0:s0 + 128, :], ob[:], reads=[obk], writes=["obuf"])

                def load_sub(si):
                    j, m = subs[si]
                    s0 = j * CH + m * 128
                    dma("sp", xb[si % 2][:], xbuf[s0:s0 + 128, :], reads=xbw_keys, writes=["xb%d" % (si % 2)])

                assert MCH >= 3
                nsub = len(subs)
                load_sub(0)
                load_sub(1)
                stage_a(0)
                load_sub(2)
                stage_a(1)
                st_next = stage_b(0)
                for si in range(nsub):
                    st_cur = st_next
                    if si + 2 < nsub:
                        stage_a(si + 2)
                    if si + 3 < nsub:
                        load_sub(si + 3)
                    if si + 1 < nsub:
                        st_next = stage_b(si + 1)
                    stage_c(si, st_cur)

                stop_at("experts")
                if last:
                    dma("sp", sh2bc[:], final_g.partition_broadcast(128), writes=["sh2bc"])
                cur_mj = None
                for ti, (r0, mj) in enumerate(moe_tiles):
                    if mj != cur_mj:
                        build_bc(g1bc, "g1bc", modT, "modT", 40, mj)
                        cur_mj = mj
                    o12c, o12ck = o12s[ti % 2], o12sk[ti % 2]
                    for k in range(2):
                        op("pool", lambda e, ti=ti, k=k, o12c=o12c: e.indirect_dma_start(
                            out=o12c[k], out_offset=None, in_=obuf[:, :], in_offset=bass.IndirectOffsetOnAxis(ap=dst[:, ti, k:k + 1], axis=0)),
                            reads=["obuf", ("dst", ti)], writes=[o12ck[k]], dma=True)
                    if ti == 0:
                        cpre = load_x(1, r0)
                    xtile, xk = cpre
                    if ti + 1 < len(moe_tiles):
                        cpre = load_x(1, moe_tiles[ti + 1][0])
                    ts(o12c[0], o12c[0], w12[:, ti, 0:1], None, ALU.mult, None, [o12ck[0], ("w12", ti)], [o12ck[0]])
                    stt(o12c[0], o12c[1], w12[:, ti, 1:2], o12c[0], ALU.mult, ALU.add, [o12ck[1], o12ck[0], ("w12", ti)], [o12ck[0]])
                    tt(o12c[0], o12c[0], g1bc[:], ALU.mult, [o12ck[0], "g1bc"], [o12ck[0]])
                    tt(xtile[:], xtile[:], o12c[0], ALU.add, [xk, o12ck[0]], [xk])
                    if not last:
                        dma("sp", xres[r0:r0 + 128, :], xtile[:], reads=[xk], writes=[("xres", r0)])
                    else:
                        rstd_of(xtile, xk, 0, h2f[:], ["h2f"])
                        stt(xtile[:], xtile[:], ss[:, 0:1], sh2bc[:], ALU.mult, ALU.mult, [xk, ("ss", 0), "sh2bc"], [xk])
                        dma("sp", out[r0:r0 + 128, :], xtile[:], reads=[xk])
        except StopBuild:
            pass
        Sx.emit()
        build.sb_bytes = Sx.sb_bytes
        build.counts = {e: len(s) for e, s in Sx.streams.items()}
    return nc


def _perm_in():
    aq, ak, av, bu, cq, ck, cv = 0, 384, 512, 640, 1152, 1536, 1664
    cols = []
    for i in range(3):
        cols += list(range(aq + i * 64, aq + i * 64 + 64)) + list(range(aq + (i + 3) * 64, aq + (i + 3) * 64 + 64))
    for i in range(3):
        cols += list(range(cq + i * 64, cq + i * 64 + 64)) + list(range(cq + (i + 3) * 64, cq + (i + 3) * 64 + 64))
    cols += list(range(ak, ak + 128))
    cols += list(range(bu, bu + 512))
    cols += list(range(ck, ck + 128))
    cols += list(range(av, av + 128)) + list(range(cv, cv + 128))
    return np.array(cols)


def _perm_out():
    rows = []
    for i in range(3):
        rows += list(range(i * 64, i * 64 + 64)) + list(range((i + 3) * 64, (i + 3) * 64 + 64))
    rows += list(range(384, 640))
    for i in range(3):
        rows += list(range(640 + i * 64, 640 + i * 64 + 64)) + list(range(640 + (i + 3) * 64, 640 + (i + 3) * 64 + 64))
    return np.array(rows)


def make_in_maps(cfg, n_cores, x, c, ctx, c_ctx, norm1_g, norm2_g, w_mod, b_mod, w_in, q_norm_g, k_norm_g,
                 conv_w, conv_b, conv_ln_g, conv_ln_b, sink, w_out, w_group, b_group,
                 w_expert, b_expert, w_gate, w_up, w_down, final_g):
    f = lambda a: np.ascontiguousarray(np.asarray(a, dtype=np.float32))
    NB, S, DEPTH = cfg.NB, cfg.S, cfg.DEPTH
    x, c, ctx, c_ctx = f(x), f(c), f(ctx), f(c_ctx)
    shared = {}
    shared["n1gT"] = f(f(norm1_g).reshape(DEPTH, 8, 128).transpose(0, 2, 1))
    shared["n2gT"] = f(f(norm2_g).reshape(DEPTH, 8, 128).transpose(0, 2, 1))
    shared["w_mod"] = f(w_mod)
    shared["bmodT"] = f(f(b_mod).reshape(DEPTH, 48, 128).transpose(0, 2, 1))
    shared["b_mod"] = f(b_mod)
    shared["w_in"] = f(f(w_in)[:, :, _perm_in()])
    gq = np.tile(f(q_norm_g), (1, 2))
    gk = np.tile(f(k_norm_g), (1, 2))
    shared["gqk"] = f(np.stack([gq, gk], axis=-1))
    cw = f(conv_w)[:, :, 0, :]
    shared["convwT"] = f(cw.reshape(DEPTH, CONV_W, 2, 128).transpose(0, 3, 2, 1))
    cpar = np.stack([f(conv_b), f(conv_ln_g), f(conv_ln_b)], axis=-1)
    shared["convp"] = f(cpar.reshape(DEPTH, 2, 128, 3).transpose(0, 2, 1, 3))
    sk = f(sink)
    sb_ = np.zeros((DEPTH, 128, 3), np.float32)
    sb_[:, :64, :] = sk[:, None, 0:3]
    sb_[:, 64:, :] = sk[:, None, 3:6]
    shared["sinkb"] = sb_
    shared["w_out"] = f(f(w_out)[:, _perm_out(), :])
    shared["wr"] = f(np.concatenate([f(w_group), f(w_expert)], axis=-1))
    shared["br"] = f(np.concatenate([f(b_group), f(b_expert)], axis=-1))
    shared["w_gate"] = f(w_gate).reshape(DEPTH * NE * 128, 8 * DE)
    shared["w_up"] = f(w_up).reshape(DEPTH * NE * 128, 8 * DE)
    shared["w_down"] = f(w_down).reshape(DEPTH * NE * 128, 4 * D)
    shared["final_g"] = f(final_g)
    for k, v in host_consts(cfg).items():
        shared["k_" + k] = v
    maps = []
    for ci in range(n_cores):
        bs = slice(ci * NB, (ci + 1) * NB)
        m = dict(shared)
        m["x_in"] = f(x[bs].reshape(NB * S, D))
        m["ctx_in"] = f(ctx[bs].reshape(NB * L, D))
        cc = np.concatenate([c[bs], c_ctx[None, :]], axis=0)
        m["cT"] = f(cc.reshape(NB + 1, 8, 128).transpose(2, 1, 0))
        maps.append(m)
    return maps


_NC_CACHE = {}


def kernel(**inputs):
    x = np.asarray(inputs["x"])
    B, S, _ = x.shape
    DEPTH = np.asarray(inputs["w_in"]).shape[0]
    n_cores = 8
    NB = B // n_cores
    cfg = Cfg(NB=NB, S=S, DEPTH=DEPTH)
    key = (NB, S, DEPTH)
    if key not in _NC_CACHE:
        _NC_CACHE[key] = build(cfg)
    nc = _NC_CACHE[key]
    maps = make_in_maps(cfg, n_cores, **inputs)
    res = run_bass_kernel_spmd(nc, maps, core_ids=list(range(n_cores)))
    outs = [np.asarray(r["out"]).reshape(NB, S, D) for r in res.results]
    return np.concatenate(outs, axis=0).astype(np.float32)
```

```python
import contextlib
from contextlib import ExitStack
import numpy as np
import concourse.bass as bass
import concourse.mybir as mybir
from concourse.bass_utils import run_bass_kernel_spmd

F32 = mybir.dt.float32
BF16 = mybir.dt.bfloat16
I32 = mybir.dt.int32
AF = mybir.ActivationFunctionType
ALU = mybir.AluOpType
AX = mybir.AxisListType

SAME_ENG_SYNC = True
COMPUTE = ("pe", "act", "dve", "pool")


class Op:
    __slots__ = ("eng", "fn", "deps", "signal", "dma", "idx", "tok")

    def __init__(self, eng, fn, dma, signal):
        self.eng = eng
        self.fn = fn
        self.deps = []
        self.signal = signal
        self.dma = dma
        self.tok = None


class Sched:
    def __init__(self, nc, es, nslots=None):
        self.nc = nc
        self.es = es
        self.streams = {e: [] for e in ("pe", "act", "dve", "pool", "sp")}
        self.nslots = nslots or {"sp": 8, "act": 4, "pool": 8}
        self.last_w = {}
        self.readers = {}
        self.sb_bytes = 0

    def sb(self, name, shape, dtype):
        n = 1
        for s in shape[1:]:
            n *= s
        self.sb_bytes += n * (2 if dtype == BF16 else 4)
        return self.es.enter_context(self.nc.sbuf_tensor(name, list(shape), dtype))

    def ps(self, name, shape, dtype):
        return self.es.enter_context(self.nc.psum_tensor(name, list(shape), dtype))

    def _target(self, op):
        if op.dma or op.signal:
            return op
        st = self.streams[op.eng]
        for j in range(op.idx + 1, len(st)):
            o = st[j]
            if (not o.dma) and o.signal:
                return o
        op.signal = True
        return op

    def op(self, eng, fn, reads=(), writes=(), dma=False, signal=True):
        o = Op(eng, fn, dma, signal)
        st = self.streams[eng]
        o.idx = len(st)
        deps = []
        for k in reads:
            w = self.last_w.get(k)
            if w is not None:
                deps.append(w)
        for k in writes:
            w = self.last_w.get(k)
            if w is not None:
                deps.append(w)
            deps.extend(self.readers.get(k, ()))
        seen = set()
        for d in deps:
            if d is o:
                continue
            if (not d.dma) and (not dma) and d.eng == eng:
                if eng == "pe" or not SAME_ENG_SYNC:
                    continue
            t = self._target(d)
            if id(t) in seen:
                continue
            seen.add(id(t))
            o.deps.append(t)
        st.append(o)
        for k in writes:
            self.last_w[k] = o
            self.readers[k] = []
        for k in reads:
            if k in writes:
                continue
            lst = self.readers.setdefault(k, [])
            if not dma:
                lst[:] = [r for r in lst if r.dma or r.eng != eng]
            lst.append(o)
        return o

    def emit(self):
        nc, es = self.nc, self.es
        sems = {}
        for e in COMPUTE:
            sems[e] = es.enter_context(nc.semaphore("s_" + e))
        for q, n in self.nslots.items():
            for s in range(n):
                sems[(q, s)] = es.enter_context(nc.semaphore("d_%s%d" % (q, s)))
        for e, st in self.streams.items():
            cnt = 0
            nd = 0
            for o in st:
                if o.dma:
                    n = self.nslots[e]
                    o.tok = ((e, nd % n), 16 * (nd // n + 1))
                    nd += 1
                elif o.signal:
                    cnt += 1
                    o.tok = (e, cnt)
        engmap = {"pe": "tensor", "act": "scalar", "dve": "vector", "pool": "gpsimd", "sp": "sync"}

        def replay(ename, eng):
            known = {}
            for o in self.streams[ename]:
                waits = {}
                for d in o.deps:
                    k, v = d.tok
                    if waits.get(k, 0) < v:
                        waits[k] = v
                if o.dma:
                    k, v = o.tok
                    if v > 16 and waits.get(k, 0) < v - 16:
                        waits[k] = v - 16
                for k, v in waits.items():
                    if known.get(k, 0) >= v:
                        continue
                    known[k] = v
                    eng.wait_ge(sems[k], v)
                ins = o.fn(eng)
                if o.dma:
                    ins.then_inc(sems[o.tok[0]], 16)
                elif o.signal:
                    ins.then_inc(sems[o.tok[0]], 1)
            if ename in self.nslots:
                last = {}
                for o in self.streams[ename]:
                    if o.dma:
                        last[o.tok[0]] = o.tok[1]
                for k, v in last.items():
                    if known.get(k, 0) < v:
                        eng.wait_ge(sems[k], v)

        with nc.Block() as block:
            for ename, attr in engmap.items():
                if not self.streams[ename]:
                    continue

                def mk(ename=ename):
                    def f(eng):
                        replay(ename, eng)
                    return f
                getattr(block, attr)(mk())


D = 1024
L = 256
HD = 64
NE = 32
DE = 512
NMOD = 6
EPS = 1e-6
CONV_W = 31
GRID_W = 64
DIN = 1792
BIG = 1.0e30


class StopBuild(Exception):
    pass


class Cfg:
    def __init__(self, NB=4, S=2048, DEPTH=4, MCH=4, debug=False, stop=None):
        self.NB, self.S, self.DEPTH, self.MCH, self.debug = NB, S, DEPTH, MCH, debug
        self.stop = stop
        self.T = NB * (S + L)
        self.NT = self.T // 128
        self.CH = 128 * MCH
        self.NCH = -(-(2 * self.T) // self.CH) + NE
        self.NSLOT = self.NCH * self.CH
        self.NKB = (S + L) // 128
        self.NQB = S // 128


def host_consts(cfg):
    S = cfg.S
    c = {}
    c["ident"] = np.eye(128, dtype=np.float32)
    sw = np.zeros((128, 128), np.float32)
    for p in range(128):
        sw[p, (p + 64) % 128] = 1.0
    c["swap"] = sw
    bo = np.zeros((128, 128), np.float32)
    bo[:64, :64] = 1.0
    bo[64:, 64:] = 1.0
    c["blockones"] = bo
    RT = np.zeros((128, 128), np.float32)
    for m in range(128):
        dd = m % 64
        half = (dd % 32) // 16
        if half == 0:
            RT[m + 16, m] = -1.0
        else:
            RT[m - 16, m] = 1.0
    c["ropeRT"] = RT
    pos = np.arange(S)
    prow = (pos // GRID_W).astype(np.float32)
    pcol = (pos % GRID_W).astype(np.float32)
    inv = (10000.0 ** (-np.arange(16, dtype=np.float32) / 16)).astype(np.float32)
    cosT = np.zeros((128, S), np.float32)
    sinT = np.zeros((128, S), np.float32)
    for p in range(128):
        dd = p % 64
        seg = dd // 32
        j = dd % 16
        ang = (prow if seg == 0 else pcol) * inv[j]
        cosT[p] = np.cos(ang)
        sinT[p] = np.sin(ang)
    c["cosT"] = cosT
    c["sinT"] = sinT
    kk = np.arange(128)[:, None]
    qq = np.arange(128)[None, :]
    mlo = (qq <= kk).astype(np.float32)
    mhi = (kk <= qq).astype(np.float32)
    c["mlo"] = np.tile(mlo, (1, 3))
    c["mhi"] = np.tile(mhi, (1, 3))
    c["tri"] = (np.arange(128)[:, None] < np.arange(128)[None, :]).astype(np.float32)
    c["jpos"] = np.tile((np.arange(cfg.NCH, dtype=np.float32) * cfg.CH)[None, :], (128, 1))
    c["pidx"] = np.arange(128, dtype=np.float32).reshape(128, 1)
    return c


CONST_SHAPES = None


def build(cfg):
    NB, S, DEPTH, T, NT, NCH, CH, MCH = cfg.NB, cfg.S, cfg.DEPTH, cfg.T, cfg.NT, cfg.NCH, cfg.CH, cfg.MCH
    NBP = NB + 1
    NKB, NQB = cfg.NKB, cfg.NQB
    SL = S + L
    nc = bass.Bass("TRN2", target_bir_lowering=False)

    def din(name, shape, dt=F32):
        return nc.dram_tensor(name, list(shape), dt, kind="ExternalInput").ap()

    x_in = din("x_in", [NB * S, D])
    ctx_in = din("ctx_in", [NB * L, D])
    cT_in = din("cT", [128, 8, NBP])
    n1gT = din("n1gT", [DEPTH, 128, 8])
    n2gT = din("n2gT", [DEPTH, 128, 8])
    w_mod = din("w_mod", [DEPTH, D, NMOD * D])
    bmodT = din("bmodT", [DEPTH, 128, 48])
    b_mod = din("b_mod", [DEPTH, NMOD * D])
    w_in = din("w_in", [DEPTH, D, DIN])
    gqk = din("gqk", [DEPTH, 128, 2])
    convwT = din("convwT", [DEPTH, 128, 2, CONV_W])
    convp = din("convp", [DEPTH, 128, 2, 3])
    sinkb = din("sinkb", [DEPTH, 128, 3])
    w_out = din("w_out", [DEPTH, D, D])
    wr = din("wr", [DEPTH, D, 36])
    br = din("br", [DEPTH, 36])
    w_gate = din("w_gate", [DEPTH * NE * 128, 8 * DE])
    w_up = din("w_up", [DEPTH * NE * 128, 8 * DE])
    w_down = din("w_down", [DEPTH * NE * 128, 4 * D])
    final_g = din("final_g", [D])
    consts = host_consts(cfg)
    cin = {k: din("k_" + k, v.shape) for k, v in consts.items()}
    out = nc.dram_tensor("out", [NB * S, D], F32, kind="ExternalOutput").ap()

    def dscr(name, shape, dt):
        return nc.dram_tensor(name, list(shape), dt, kind="Internal").ap()

    xres = dscr("xres", [T, D], F32)
    h2buf = dscr("h2buf", [T, D], BF16)
    xbuf = dscr("xbuf", [cfg.NSLOT, D], BF16)
    obuf = dscr("obuf", [cfg.NSLOT, D], F32)
    modrows = dscr("modrows", [NBP, 4 * D], F32)
    dbg = {}
    if cfg.debug:
        dbg["xmid"] = nc.dram_tensor("dbg_xmid", [T, D], F32, kind="ExternalOutput").ap()
        dbg["route"] = nc.dram_tensor("dbg_route", [128, NT, 6], F32, kind="ExternalOutput").ap()
        dbg["h2"] = nc.dram_tensor("dbg_h2", [T, D], F32, kind="ExternalOutput").ap()
        dbg["bc"] = nc.dram_tensor("dbg_bc", [3, 128, D], F32, kind="ExternalOutput").ap()
        dbg["ss"] = nc.dram_tensor("dbg_ss", [NT, 128, 4], F32, kind="ExternalOutput").ap()
        dbg["rt"] = nc.dram_tensor("dbg_rt", [NT, 128, 64], F32, kind="ExternalOutput").ap()

    with ExitStack() as es:
        Sx = Sched(nc, es)
        sb, op = Sx.sb, Sx.op

        def dma(q, o, i, reads=(), writes=()):
            op(q, lambda e: e.dma_start(out=o, in_=i), reads=reads, writes=writes, dma=True)

        def mm(o, lhsT, rhs, start, stop, reads, writes, signal=True):
            op("pe", lambda e: e.matmul(o, lhsT=lhsT, rhs=rhs, start=start, stop=stop), reads=reads, writes=writes, signal=signal)

        def tr(o, i, ident, reads, writes, signal=True):
            op("pe", lambda e: e.transpose(out=o, in_=i, identity=ident), reads=reads, writes=writes, signal=signal)

        def act(o, i, func, reads, writes, scale=1.0, bias=0.0, accum_out=None):
            if accum_out is None:
                op("act", lambda e: e.activation(out=o, in_=i, func=func, bias=bias, scale=scale), reads=reads, writes=writes)
            else:
                op("act", lambda e: e.activation(out=o, in_=i, func=func, bias=bias, scale=scale, accum_out=accum_out), reads=reads, writes=writes)

        def tt(o, a, b, alu, reads, writes, eng="dve"):
            op(eng, lambda e: e.tensor_tensor(out=o, in0=a, in1=b, op=alu), reads=reads, writes=writes)

        def ts(o, a, s1, s2, op0, op1, reads, writes, eng="dve"):
            if op1 is None:
                op(eng, lambda e: e.tensor_scalar(out=o, in0=a, scalar1=s1, scalar2=None, op0=op0), reads=reads, writes=writes)
            else:
                op(eng, lambda e: e.tensor_scalar(out=o, in0=a, scalar1=s1, scalar2=s2, op0=op0, op1=op1), reads=reads, writes=writes)

        def stt(o, a, s, b, op0, op1, reads, writes, eng="dve"):
            op(eng, lambda e: e.scalar_tensor_tensor(out=o, in0=a, scalar=s, in1=b, op0=op0, op1=op1), reads=reads, writes=writes)

        def cp(o, i, reads, writes, eng="dve"):
            op(eng, lambda e: e.tensor_copy(out=o, in_=i), reads=reads, writes=writes)

        def recip(o, i, reads, writes):
            op("dve", lambda e: e.reciprocal(out=o, in_=i), reads=reads, writes=writes)

        def red(o, i, alu, reads, writes):
            op("dve", lambda e: e.tensor_reduce(out=o, in_=i, axis=AX.X, op=alu), reads=reads, writes=writes)

        def memset(o, v, writes, eng="dve"):
            op(eng, lambda e: e.memset(o, v), writes=writes)

        banks = [Sx.ps("pb%d" % i, [128, 512], F32) for i in range(8)]

        class Rot:
            def __init__(self, ids):
                self.ids, self.i = ids, 0

            def next(self):
                b = self.ids[self.i % len(self.ids)]
                self.i += 1
                return banks[b], "pb%d" % b

        rot_tr = Rot([0, 1])
        rot_mm = Rot([2, 3])
        rot_acc = Rot([4, 5])
        rot_s = Rot([6, 7, 3, 2])
        rot_d = Rot([6, 7])
        att_ctr = [0]

        def cf32(name, shape):
            t = sb("c_" + name, shape, F32)
            dma("sp", t[:], cin[name], writes=["c_" + name])
            return t

        def cbf(name, shape):
            t = sb("cb_" + name, shape, BF16)
            dma("pool", t[:], cin[name], writes=["cb_" + name])
            return t

        identf = cf32("ident", [128, 128])
        identb = cbf("ident", [128, 128])
        swapf = cf32("swap", [128, 128])
        blockones_b = cbf("blockones", [128, 128])
        ropeRT_b = cbf("ropeRT", [128, 128])
        cosT = cbf("cosT", [128, S])
        sinT = cbf("sinT", [128, S])
        mlo_b = cbf("mlo", [128, 384])
        mhi_b = cbf("mhi", [128, 384])
        trif = cf32("tri", [128, 128])
        jpos = cf32("jpos", [128, NCH])
        pidx = cf32("pidx", [128, 1])
        onesf = sb("onesf", [128, 128], F32)
        memset(onesf[:], 1.0, ["onesf"])
        onesb = sb("onesb", [128, 128], BF16)
        memset(onesb[:], 1.0, ["onesb"])
        zerosf = sb("zerosf", [128, 128], F32)
        memset(zerosf[:], 0.0, ["zerosf"])
        scT = sb("scT", [128, 8, NBP], F32)
        dma("sp", scT[:], cT_in, writes=["scT"])
        act(scT[:], scT[:], AF.Silu, ["scT"], ["scT"])

        wq = sb("wq", [128, 8, 768], BF16)
        wkvo = sb("wkvo", [128, 8, 1024], BF16)
        wrt = sb("wrt", [128, 8, 36], F32)
        brt = sb("brt", [128, 36], F32)
        modT = sb("modT", [128, 48, NBP], F32)
        G2c = sb("G2c", [128, 8, NBP], F32)
        n2T = sb("n2T", [128, 8], F32)
        dg = [sb("dg%d" % i, [128, 128], F32) for i in range(2)]
        bmT = sb("bmT", [128, 48], F32)
        n1T = sb("n1T", [128, 8], F32)
        G1 = sb("G1", [128, 8, NBP], F32)
        gqk_t = sb("gqk_t", [128, 2], F32)
        cw_t = sb("cw_t", [128, 2, CONV_W], F32)
        cpar = sb("cpar", [128, 2, 3], F32)
        diagw = sb("diagw", [128, CONV_W, 128], BF16)
        esk3 = sb("esk3", [128, 3], F32)
        esink_t = sb("esink_t", [128, 3, 128], F32)
        WMC = 256
        g1bc = sb("g1bc", [128, D], F32)
        G2bc = sb("G2bc", [128, D], F32)
        sh2bc = sb("sh2bc", [128, D], F32)
        ARN = max(8 * DE, NKB * 256, 2 * SL, 2 * (S + 30) + 2 * (L + 30))
        AR = [sb("AR%d" % i, [128, ARN], BF16) for i in range(6)]
        VA = AR[0][:, 0:NKB * 256].rearrange("p (k g c) -> p k g c", g=2, c=128)
        VC = AR[1][:, 0:NKB * 256].rearrange("p (k g c) -> p k g c", g=2, c=128)
        kA = AR[2][:, 0:2 * SL].rearrange("p (g t) -> p g t", g=2)
        kC = AR[3][:, 0:2 * SL].rearrange("p (g t) -> p g t", g=2)
        hgl = AR[4][:, 0:2 * (S + 30)].rearrange("p (c t) -> p c t", c=2)
        hglc = AR[4][:, 2 * (S + 30):2 * (S + 30) + 2 * (L + 30)].rearrange("p (c t) -> p c t", c=2)
        cvo = AR[5][:, 0:2 * SL].rearrange("p (c t) -> p c t", c=2)
        wg = [AR[0][:, 0:8 * DE], AR[3][:, 0:8 * DE]]
        wu = [AR[1][:, 0:8 * DE], AR[4][:, 0:8 * DE]]
        wd = [AR[2][:, 0:4 * D], AR[5][:, 0:4 * D]]
        wgk, wuk, wdk = ["AR0", "AR3"], ["AR1", "AR4"], ["AR2", "AR5"]
        wmv = [AR[i][:, 0:4096].bitcast(F32).rearrange("p (c n) -> p c n", c=8) for i in range(4)]
        o12s = [[AR[2 * i + k][:, 0:2048].bitcast(F32) for k in range(2)] for i in range(2)]
        o12sk = [["AR%d" % (2 * i + k) for k in range(2)] for i in range(2)]
        qA = sb("qA", [128, 3, 512], BF16)
        qC = sb("qC", [128, 4, 3, 128], BF16)
        mixc = sb("mixc", [128, 6, 512], BF16)
        xt = [sb("xt%d" % i, [128, D], F32) for i in range(2)]
        xn = sb("xn", [128, 4, D], BF16)
        hT = sb("hT", [128, 8, 512], BF16)
        ss = sb("ss", [128, 4], F32)
        t512 = [sb("t512_%d" % i, [128, 512], F32) for i in range(4)]
        b512 = [sb("b512_%d" % i, [128, 512], BF16) for i in range(3)]
        pT = [sb("pT%d" % i, [128, 512], BF16) for i in range(3)]
        o12 = [xn[:, 2 * k:2 * k + 2, :].rearrange("p a b -> p (a b)").bitcast(F32) for k in range(2)]
        o12k = [[("xn", 0), ("xn", 1)], [("xn", 2), ("xn", 3)]]
        h2T = hT[:, 0:4, :].rearrange("p a b -> p (a b)").bitcast(F32).rearrange("p (c t) -> p c t", t=128)
        h2Tk = [("hT", c) for c in range(4)]
        O1a = sb("O1a", [128, NT, NE], BF16)
        O2a = sb("O2a", [128, NT, NE], BF16)
        r12 = sb("r12", [128, NT, 2], F32)
        w12 = sb("w12", [128, NT, 2], F32)
        dst = sb("dst", [128, NT, 2], I32)
        cum = sb("cum", [128, NE], F32)
        rt = sb("rt", [128, 64], F32)
        rtb = sb("rtb", [128, 4, NE], F32)
        pst = sb("pst", [128, NE + 1], F32)
        pad_i = sb("pad_i", [128, NE], I32)
        cexp = sb("cexp", [128, NCH], F32)
        ctmp = sb("ctmp", [128, NCH], F32)
        gidx = sb("gidx", [128, NCH], I32)
        h2f = sb("h2f", [128, D], F32)
        h2fs = [h2f[:], xn[:, 0:2, :].rearrange("p a b -> p (a b)").bitcast(F32)]
        h2fsk = [["h2f"], [("xn", 0), ("xn", 1)]]
        xb = [sb("xb%d" % i, [128, D], BF16) for i in range(2)]
        xbT2 = [sb("xbT%d" % i, [128, 8, 128], BF16) for i in range(2)]
        a_sb2 = [sb("a_sb%d" % i, [128, DE], BF16) for i in range(2)]
        aT2 = [sb("aT%d" % i, [128, 4, 128], BF16) for i in range(2)]
        ob2 = [h2f, sb("ob1", [128, D], F32)]
        ob2k = ["h2f", "ob1"]

        def segs_of(b):
            return [("lat", b, b * S, S, b), ("ctx", b, NB * S + b * L, L, NB)]

        def src_rows(l, row0, n):
            if l == 0:
                if row0 < NB * S:
                    return x_in[row0:row0 + n, :]
                return ctx_in[row0 - NB * S:row0 - NB * S + n, :]
            return xres[row0:row0 + n, :]

        xti = [0]

        def load_x(l, row0):
            i = xti[0] % 2
            xti[0] += 1
            dma("sp", xt[i][:], src_rows(l, row0, 128), reads=[("xres", row0)], writes=["xt%d" % i])
            return xt[i], "xt%d" % i

        def rstd_of(xtile, xk, col, junk_ap, junk_k):
            act(junk_ap, xtile[:], AF.Square, [xk], list(junk_k) + [("ss", col)], accum_out=ss[:, col:col + 1])
            act(ss[:, col:col + 1], ss[:, col:col + 1], AF.Ln, [("ss", col)], [("ss", col)], scale=1.0 / D, bias=EPS)
            act(ss[:, col:col + 1], ss[:, col:col + 1], AF.Exp, [("ss", col)], [("ss", col)], scale=-0.5)

        def make_hT(l, row0, W, mj):
            ntl = W // 128
            for ti in range(ntl):
                xtile, xk = load_x(l, row0 + ti * 128)
                rstd_of(xtile, xk, ti, xn[:, ti, :], [("xn", ti)])
                ts(xn[:, ti, :], xtile[:], ss[:, ti:ti + 1], None, ALU.mult, None, [xk, ("ss", ti)], [("xn", ti)])
            for c in range(8):
                ps, pk = rot_tr.next()
                psb = ps[:].bitcast(BF16)
                for ti in range(ntl):
                    tr(psb[:, ti * 128:(ti + 1) * 128], xn[:, ti, c * 128:(c + 1) * 128], identb[:], [("xn", ti), "cb_ident"], [pk], signal=(ti == ntl - 1))
                act(hT[:, c, 0:W], psb[:, 0:W], AF.Identity, [pk, "G1", "modT"], [("hT", c)], scale=G1[:, c, mj:mj + 1], bias=modT[:, c, mj:mj + 1])

        hTk = [("hT", c) for c in range(8)]

        def proj(wt, wk, c0, W):
            ps, pk = rot_mm.next()
            for c in range(8):
                mm(ps[:, 0:W], wt[:, c, c0:c0 + 128], hT[:, c, 0:W], c == 0, c == 7, [wk] + hTk, [pk], signal=(c == 7))
            return ps, pk

        def rope_store(src, srck, W, tcols, dst_fn):
            ps, pk = rot_tr.next()
            mm(ps[:, 0:W], ropeRT_b[:], src[:, 0:W], True, True, ["cb_ropeRT", srck], [pk])
            tt(t512[0][:, 0:W], src[:, 0:W], cosT[:, tcols], ALU.mult, [srck, "cb_cosT"], ["t512_0"])
            tt(t512[1][:, 0:W], ps[:, 0:W], sinT[:, tcols], ALU.mult, [pk, "cb_sinT"], ["t512_1"])
            dst_fn(t512[0], t512[1])

        def qk_norm(ps, pk, gcol, W):
            act(b512[0][:, 0:W], ps[:, 0:W], AF.Square, [pk], ["b512_0"])
            p2, p2k = rot_tr.next()
            mm(p2[:, 0:W], blockones_b[:], b512[0][:, 0:W], True, True, ["cb_blockones", "b512_0"], [p2k])
            act(t512[2][:, 0:W], p2[:, 0:W], AF.Ln, [p2k], ["t512_2"], scale=1.0 / HD, bias=EPS)
            act(t512[2][:, 0:W], t512[2][:, 0:W], AF.Exp, ["t512_2"], ["t512_2"], scale=-0.5)
            stt(b512[1][:, 0:W], ps[:, 0:W], gqk_t[:, gcol:gcol + 1], t512[2][:, 0:W], ALU.mult, ALU.mult, [pk, "gqk_t", "t512_2"], ["b512_1"])
            return b512[1], "b512_1"

        def attention(q_ap, qk_, W, keyblocks, kT, kTk, V, Vk, dst_top, dst_bot, sink):
            nk = len(keyblocks)
            steps = [(g, idx) for g in range(2) for idx in range(nk)]
            accs = [rot_acc.next(), rot_acc.next()]
            LA = 3

            def qk_mm(sidx):
                g, idx = steps[sidx]
                kb = keyblocks[idx][0]
                ps, pk = rot_s.next()
                mm(ps[:, 0:W], kT[:, g, kb * 128:(kb + 1) * 128], q_ap, True, True, [kTk, qk_], [pk])
                return ps, pk

            issued = [qk_mm(i) for i in range(min(LA, len(steps)))]
            for sidx, (g, idx) in enumerate(steps):
                kb, mask, mk_ = keyblocks[idx]
                ps, pk = issued[sidx]
                acc, ak = accs[g]
                pi = att_ctr[0] % len(pT)
                att_ctr[0] += 1
                act(pT[pi][:, 0:W], ps[:, 0:W], AF.Exp, [pk], ["pT%d" % pi], scale=HD ** -0.5)
                if mask is not None:
                    tt(pT[pi][:, 0:W], pT[pi][:, 0:W], mask[:, 0:W], ALU.mult, ["pT%d" % pi, mk_], ["pT%d" % pi])
                mm(acc[:, 0:W], V[:, kb, g, :], pT[pi][:, 0:W], idx == 0, idx == nk - 1, [Vk, "pT%d" % pi], [ak], signal=(idx == nk - 1))
                if sidx + LA < len(steps):
                    issued.append(qk_mm(sidx + LA))
            (a0, a0k), (a1, a1k) = accs
            R = t512[2]
            act(R[64:128, 0:W], a0[64:128, 0:W], AF.Identity, [a0k], ["t512_2"])
            act(R[0:64, 0:W], a1[0:64, 0:W], AF.Identity, [a1k], ["t512_2"])
            ps, pk = rot_s.next()
            mm(ps[:, 0:W], swapf[:], R[:, 0:W], True, True, ["c_swap", "t512_2"], [pk])
            if sink:
                tt(t512[3][:, 0:W], ps[:, 0:W], esink_t[:].rearrange("p a b -> p (a b)")[:, 0:W], ALU.add, [pk, "esink_t"], ["t512_3"])
                act(t512[3][:, 0:W], t512[3][:, 0:W], AF.Ln, ["t512_3"], ["t512_3"])
            else:
                act(t512[3][:, 0:W], ps[:, 0:W], AF.Ln, [pk], ["t512_3"])
            act(t512[3][:, 0:W], t512[3][:, 0:W], AF.Exp, ["t512_3"], ["t512_3"], scale=-1.0)
            dst_top(a0, a0k, t512[3])
            dst_bot(a1, a1k, t512[3])

        dgi = [0]

        def build_bc(dst_t, dk, src_t, srck, c_off, j):
            for half in range(2):
                ps, pk = rot_mm.next()
                for c4 in range(4):
                    c = half * 4 + c4
                    di = dgi[0] % 2
                    dgi[0] += 1
                    ts(dg[di][:], identf[:], src_t[:, c_off + c, j:j + 1], None, ALU.mult, None, ["c_ident", srck], ["dg%d" % di])
                    mm(ps[:, c4 * 128:(c4 + 1) * 128], onesf[:], dg[di][:], True, True, ["onesf", "dg%d" % di], [pk])
                act(dst_t[:, half * 512:(half + 1) * 512], ps[:, :], AF.Identity, [pk], [dk])

        def stop_at(name):
            if cfg.stop == name:
                raise StopBuild()

        try:
            for l in range(DEPTH):
                last = (l == DEPTH - 1)
                w_in_v = w_in[l].rearrange("(c p) n -> p c n", p=128)
                dma("pool", wq[:], w_in_v[:, :, 0:768], writes=["wq"])
                dma("sp", wrt[:], wr[l].rearrange("(c p) n -> p c n", p=128), writes=["wrt"])
                dma("sp", brt[:], br[l].partition_broadcast(128), writes=["brt"])
                dma("sp", bmT[:], bmodT[l], writes=["bmT"])
                dma("sp", n1T[:], n1gT[l], writes=["n1T"])
                dma("sp", n2T[:], n2gT[l], writes=["n2T"])
                dma("sp", gqk_t[:], gqk[l], writes=["gqk_t"])
                dma("sp", cw_t[:], convwT[l], writes=["cw_t"])
                dma("sp", cpar[:], convp[l], writes=["cpar"])
                dma("sp", esk3[:], sinkb[l], writes=["esk3"])
                act(esk3[:], esk3[:], AF.Exp, ["esk3"], ["esk3"])
                for hh in range(3):
                    ts(esink_t[:, hh, :], zerosf[:, :], esk3[:, hh:hh + 1], None, ALU.add, None, ["zerosf", "esk3"], ["esink_t"])
                stop_at("params")
                stop_at("arena")
                wmod_v = w_mod[l].rearrange("(c p) n -> p c n", p=128)
                npieces = (6 * D) // WMC
                for piece in range(npieces):
                    wt_, wk = wmv[piece % 4], "AR%d" % (piece % 4)
                    dma("sp", wt_, wmod_v[:, :, piece * WMC:(piece + 1) * WMC], writes=[wk])
                    for f in range(WMC // 128):
                        fc = piece * (WMC // 128) + f
                        ps, pk = rot_mm.next()
                        for c in range(8):
                            mm(ps[:, 0:NBP], wt_[:, c, f * 128:(f + 1) * 128], scT[:, c, :], c == 0, c == 7, [wk, "scT"], [pk], signal=(c == 7))
                        act(modT[:, fc, :], ps[:, 0:NBP], AF.Identity, [pk, "bmT"], ["modT"], bias=bmT[:, fc:fc + 1])
                for c in range(8):
                    ts(G1[:, c, :], modT[:, 8 + c, :], 1.0, n1T[:, c:c + 1], ALU.add, ALU.mult, ["modT", "n1T"], ["G1"])
                    ts(G2c[:, c, :], modT[:, 32 + c, :], 1.0, n2T[:, c:c + 1], ALU.add, ALU.mult, ["modT", "n2T"], ["G2c"])

                memset(AR[0][:], 1.0, ["AR0"])
                memset(AR[1][:], 1.0, ["AR1"])
                memset(AR[2][:], 0.0, ["AR2"])
                memset(AR[3][:], 0.0, ["AR3"])
                memset(AR[4][:], 0.0, ["AR4"], eng="pool")
                stop_at("mod")
                memset(cum[:], 0.0, ["cum"])
                tile_ctr = [0]
                moe_tiles = []
                for b in range(NB):
                    dma("pool", wkvo[:], w_in_v[:, :, 768:DIN], reads=[], writes=["wkvo"])
                    for (kind, _, row0, ntok, mj) in segs_of(b):
                        is_lat = kind == "lat"
                        col_base = 0 if is_lat else S
                        for g0 in range(0, ntok, 512):
                            W = min(512, ntok - g0)
                            ntl = W // 128
                            make_hT(l, row0 + g0, W, mj)
                            cols = slice(col_base + g0, col_base + g0 + W)
                            tcols = slice(g0, g0 + W)
                            nb0 = (col_base + g0) // 128
                            stop_at("hT")
                            ps, pk = proj(wkvo, "wkvo", 0, W)
                            kn, knk = qk_norm(ps, pk, 1, W)
                            if is_lat:
                                def kst(a, bb, cols=cols, W=W):
                                    tt(kA[0:64, 0, cols], a[0:64, 0:W], bb[0:64, 0:W], ALU.add, ["t512_0", "t512_1"], ["AR2"])
                                    tt(kA[64:128, 1, cols], a[64:128, 0:W], bb[64:128, 0:W], ALU.add, ["t512_0", "t512_1"], ["AR2"])
                                rope_store(kn, knk, W, tcols, kst)
                            else:
                                cp(kA[0:64, 0, cols], kn[0:64, 0:W], [knk], ["AR2"])
                                cp(kA[64:128, 1, cols], kn[64:128, 0:W], [knk], ["AR2"])
                            stop_at("ak")
                            hg = hgl if is_lat else hglc
                            for cc in range(2):
                                psg, pgk = proj(wkvo, "wkvo", (3 + cc) * 128, W)
                                act(t512[3][:, 0:W], psg[:, 0:W], AF.Sigmoid, [pgk], ["t512_3"])
                                psa, pak = proj(wkvo, "wkvo", (1 + cc) * 128, W)
                                tt(hg[:, cc, 15 + g0:15 + g0 + W], psa[:, 0:W], t512[3][:, 0:W], ALU.mult, [pak, "t512_3"], ["AR4"])
                            stop_at("glu")
                            ps, pk = proj(wkvo, "wkvo", 5 * 128, W)
                            if is_lat:
                                act(b512[2][:, 0:W], ps[:, 0:W], AF.Identity, [pk], ["b512_2"])

                                def kcst(a, bb, cols=cols, W=W):
                                    tt(kC[0:64, 0, cols], a[0:64, 0:W], bb[0:64, 0:W], ALU.add, ["t512_0", "t512_1"], ["AR3"])
                                    tt(kC[64:128, 1, cols], a[64:128, 0:W], bb[64:128, 0:W], ALU.add, ["t512_0", "t512_1"], ["AR3"])
                                rope_store(b512[2], "b512_2", W, tcols, kcst)
                            else:
                                act(kC[0:64, 0, cols], ps[0:64, 0:W], AF.Identity, [pk], ["AR3"])
                                act(kC[64:128, 1, cols], ps[64:128, 0:W], AF.Identity, [pk], ["AR3"])
                            stop_at("ck")
                            for ti in range(ntl):
                                kb = nb0 + ti
                                ps, pk = rot_mm.next()
                                for c in range(8):
                                    mm(ps[:, 0:256], hT[:, c, ti * 128:(ti + 1) * 128], wkvo[:, c, 768:1024], c == 0, c == 7, ["wkvo"] + hTk, [pk], signal=(c == 7))
                                cp(VA[:, kb, 0, 0:64], ps[:, 0:64], [pk], ["AR0"])
                                cp(VA[:, kb, 1, 64:128], ps[:, 64:128], [pk], ["AR0"])
                                cp(VC[:, kb, 0, 0:64], ps[:, 128:192], [pk], ["AR1"])
                                cp(VC[:, kb, 1, 64:128], ps[:, 192:256], [pk], ["AR1"])
                            stop_at("v1")
                    stop_at("pass1")
                    dma("pool", wkvo[:], w_out[l].rearrange("(c p) n -> p c n", p=128), writes=["wkvo"])
                    conv_segs = [(hgl, S, 0)] + ([] if last else [(hglc, L, S)])
                    for cc in range(2):
                        for j in range(CONV_W):
                            ts(diagw[:, j, :], identf[:, :], cw_t[:, cc, j:j + 1], None, ALU.mult, None, ["c_ident", "cw_t"], ["diagw"])
                        for (hg, n_tok, cb) in conv_segs:
                            for g0 in range(0, n_tok, 512):
                                W = min(512, n_tok - g0)
                                ps, pk = rot_mm.next()
                                for j in range(CONV_W):
                                    mm(ps[:, 0:W], diagw[:, j, :], hg[:, cc, g0 + j:g0 + j + W], j == 0, j == CONV_W - 1, ["diagw", "AR4"], [pk], signal=(j == CONV_W - 1))
                                act(cvo[:, cc, cb + g0:cb + g0 + W], ps[:, 0:W], AF.Identity, [pk, "cpar"], ["AR5"], bias=cpar[:, cc, 0:1])
                    for (hg, n_tok, cb) in conv_segs:
                        for g0 in range(0, n_tok, 512):
                            W = min(512, n_tok - g0)
                            cs = slice(cb + g0, cb + g0 + W)
                            p1, p1k = rot_tr.next()
                            for cc in range(2):
                                mm(p1[:, 0:W], onesb[:], cvo[:, cc, cs], cc == 0, cc == 1, ["onesb", "AR5"], [p1k], signal=(cc == 1))
                            act(t512[0][:, 0:W], p1[:, 0:W], AF.Identity, [p1k], ["t512_0"], scale=1.0 / 256)
                            p2, p2k = rot_tr.next()
                            for cc in range(2):
                                act(b512[cc][:, 0:W], cvo[:, cc, cs], AF.Square, ["AR5"], ["b512_%d" % cc])
                                mm(p2[:, 0:W], onesb[:], b512[cc][:, 0:W], cc == 0, cc == 1, ["onesb", "b512_%d" % cc], [p2k], signal=(cc == 1))
                            tt(t512[1][:, 0:W], t512[0][:, 0:W], t512[0][:, 0:W], ALU.mult, ["t512_0"], ["t512_1"])
                            stt(t512[2][:, 0:W], p2[:, 0:W], 1.0 / 256, t512[1][:, 0:W], ALU.mult, ALU.subtract, [p2k, "t512_1"], ["t512_2"])
                            act(t512[2][:, 0:W], t512[2][:, 0:W], AF.Ln, ["t512_2"], ["t512_2"], bias=EPS)
                            act(t512[2][:, 0:W], t512[2][:, 0:W], AF.Exp, ["t512_2"], ["t512_2"], scale=-0.5)
                            for cc in range(2):
                                tt(t512[3][:, 0:W], cvo[:, cc, cs], t512[0][:, 0:W], ALU.subtract, ["AR5", "t512_0"], ["t512_3"])
                                tt(t512[3][:, 0:W], t512[3][:, 0:W], t512[2][:, 0:W], ALU.mult, ["t512_3", "t512_2"], ["t512_3"])
                                act(cvo[:, cc, cs], t512[3][:, 0:W], AF.Silu, ["t512_3", "cpar"], ["AR5"], scale=cpar[:, cc, 1:2], bias=cpar[:, cc, 2:3])

                    stop_at("conv")
                    ctxkeys = [(NQB, None, None), (NQB + 1, None, None)]
                    allkeys = [(kb, None, None) for kb in range(NKB)]
                    for (kind, _, row0, ntok, mj) in segs_of(b):
                        is_lat = kind == "lat"
                        if last and not is_lat:
                            continue
                        col_base = 0 if is_lat else S
                        build_bc(g1bc, "g1bc", modT, "modT", 16, mj)
                        build_bc(sh2bc, "sh2bc", modT, "modT", 24, mj)
                        build_bc(G2bc, "G2bc", G2c, "G2c", 0, mj)
                        if cfg.debug and l == 0 and b == 0 and is_lat:
                            dma("sp", dbg["bc"][0], g1bc[:], reads=["g1bc"])
                            dma("sp", dbg["bc"][1], sh2bc[:], reads=["sh2bc"])
                            dma("sp", dbg["bc"][2], G2bc[:], reads=["G2bc"])
                        for g0 in range(0, ntok, 512):
                            W = min(512, ntok - g0)
                            ntl = W // 128
                            make_hT(l, row0 + g0, W, mj)
                            tcols = slice(g0, g0 + W)
                            for i in range(3):
                                ps, pk = proj(wq, "wq", i * 128, W)
                                qn, qnk = qk_norm(ps, pk, 0, W)
                                if is_lat:
                                    rope_store(qn, qnk, W, tcols, lambda a, bb, i=i, W=W: tt(qA[:, i, 0:W], a[:, 0:W], bb[:, 0:W], ALU.add, ["t512_0", "t512_1"], ["qA"]))
                                else:
                                    cp(qA[:, i, 0:W], qn[:, 0:W], [qnk], ["qA"])
                            for i in range(3):
                                ps, pk = proj(wq, "wq", (3 + i) * 128, W)
                                if is_lat:
                                    act(b512[2][:, 0:W], ps[:, 0:W], AF.Identity, [pk], ["b512_2"])
                                    rope_store(b512[2], "b512_2", W, tcols, lambda a, bb, i=i, W=W, ntl=ntl: tt(
                                        qC[:, 0:ntl, i, :], a[:, 0:W].rearrange("p (n q) -> p n q", q=128), bb[:, 0:W].rearrange("p (n q) -> p n q", q=128),
                                        ALU.add, ["t512_0", "t512_1"], ["qC"]))
                                else:
                                    act(qC[:, 0:ntl, i, :], ps[:, 0:W].rearrange("p (n q) -> p n q", q=128), AF.Identity, [pk], ["qC"])
                            keysA = allkeys if is_lat else ctxkeys
                            for i in range(3):
                                attention(qA[:, i, 0:W], "qA", W, keysA, kA, "AR2", VA, "AR0",
                                          lambda a, ak, rc, i=i, W=W: tt(mixc[0:64, i, 0:W], a[0:64, 0:W], rc[0:64, 0:W], ALU.mult, [ak, "t512_3"], ["mixc"]),
                                          lambda a, ak, rc, i=i, W=W: tt(mixc[64:128, i, 0:W], a[64:128, 0:W], rc[64:128, 0:W], ALU.mult, [ak, "t512_3"], ["mixc"]),
                                          False)
                            def attn_c(bi, g0=g0, is_lat=is_lat):
                                if is_lat:
                                    n = g0 // 128 + bi
                                    kbs = []
                                    if n > 0:
                                        kbs.append((n - 1, mlo_b, "cb_mlo"))
                                    kbs.append((n, None, None))
                                    if n < NQB - 1:
                                        kbs.append((n + 1, mhi_b, "cb_mhi"))
                                    kbs += ctxkeys
                                else:
                                    kbs = ctxkeys
                                c0 = bi * 128
                                attention(qC[:, bi, :, :].rearrange("p a b -> p (a b)"), "qC", 384, kbs, kC, "AR3", VC, "AR1",
                                          lambda a, ak, rc, c0=c0: tt(mixc[0:64, 3:6, c0:c0 + 128], a[0:64, 0:384].rearrange("p (a b) -> p a b", b=128),
                                                                      rc[0:64, 0:384].rearrange("p (a b) -> p a b", b=128), ALU.mult, [ak, "t512_3"], [("mixcC", c0)]),
                                          lambda a, ak, rc, c0=c0: tt(mixc[64:128, 3:6, c0:c0 + 128], a[64:128, 0:384].rearrange("p (a b) -> p a b", b=128),
                                                                      rc[64:128, 0:384].rearrange("p (a b) -> p a b", b=128), ALU.mult, [ak, "t512_3"], [("mixcC", c0)]),
                                          True)
                            pre = {0: load_x(l, row0 + g0)}

                            def tile_gen(tl, pre=pre, row0=row0, g0=g0, ntl=ntl, col_base=col_base, mj=mj):
                                r0 = row0 + g0 + tl * 128
                                cg = col_base + g0 + tl * 128
                                xtile, xk = pre[tl]
                                if tl + 1 < ntl:
                                    pre[tl + 1] = load_x(l, r0 + 128)
                                hb, hbk = h2fs[tl % 2], h2fsk[tl % 2]
                                sc_ = tl % 4
                                attn_c(tl)
                                ck_ = ("mixcC", tl * 128)
                                lhs = [(mixc[:, 0, tl * 128:(tl + 1) * 128], "mixc"), (mixc[:, 1, tl * 128:(tl + 1) * 128], "mixc"), (mixc[:, 2, tl * 128:(tl + 1) * 128], "mixc"),
                                       (cvo[:, 0, cg:cg + 128], "AR5"), (cvo[:, 1, cg:cg + 128], "AR5"),
                                       (mixc[:, 3, tl * 128:(tl + 1) * 128], ck_), (mixc[:, 4, tl * 128:(tl + 1) * 128], ck_), (mixc[:, 5, tl * 128:(tl + 1) * 128], ck_)]
                                for half in range(2):
                                    hs = slice(half * 512, (half + 1) * 512)
                                    ps, pk = rot_mm.next()
                                    for c in range(8):
                                        mm(ps[:, :], lhs[c][0], wkvo[:, c, hs], c == 0, c == 7, [lhs[c][1], "wkvo"], [pk], signal=(c == 7))
                                    tt(t512[half][:, :], ps[:, :], g1bc[:, hs], ALU.mult, [pk, "g1bc"], ["t512_%d" % half])
                                    tt(xtile[:, hs], xtile[:, hs], t512[half][:, :], ALU.add, [xk, "t512_%d" % half], [xk])
                                dma("sp", xres[r0:r0 + 128, :], xtile[:], reads=[xk], writes=[("xres", r0)])
                                if cfg.debug and l == 0:
                                    dma("sp", dbg["xmid"][r0:r0 + 128, :], xtile[:], reads=[xk])
                                rstd_of(xtile, xk, sc_, hb, hbk)
                                stt(hb, xtile[:], ss[:, sc_:sc_ + 1], G2bc[:], ALU.mult, ALU.mult, [xk, ("ss", sc_), "G2bc"], hbk)
                                tt(hb, hb, sh2bc[:], ALU.add, hbk + ["sh2bc"], hbk)
                                yield
                                ti = tile_ctr[0]
                                tile_ctr[0] += 1
                                moe_tiles.append((r0, mj))
                                act(xb[0][:], hb, AF.Identity, hbk, ["xb0"])
                                if cfg.debug and l == 0:
                                    dma("sp", dbg["h2"][r0:r0 + 128, :], hb, reads=hbk)
                                    dma("sp", dbg["ss"][ti], ss[:, :], reads=[("ss", sc_)])
                                dma("sp", h2buf[r0:r0 + 128, :], xb[0][:], reads=["xb0"], writes=[("h2buf", r0)])
                                for hf in range(2):
                                    ps, pk = rot_tr.next()
                                    for c4 in range(4):
                                        c = hf * 4 + c4
                                        tr(ps[:, c4 * 128:(c4 + 1) * 128], hb[:, c * 128:(c + 1) * 128], identf[:], hbk + ["c_ident"], [pk], signal=(c4 == 3))
                                    act(h2T[:, hf * 4:hf * 4 + 4, :], ps[:, :].rearrange("p (a b) -> p a b", b=128), AF.Identity, [pk], h2Tk)
                                ps, pk = rot_mm.next()
                                for c in range(8):
                                    mm(ps[:, 0:36], h2T[:, c, :], wrt[:, c, :], c == 0, c == 7, h2Tk + ["wrt"], [pk], signal=(c == 7))
                                RT_ = "rt"
                                tt(rt[:, 0:36], ps[:, 0:36], brt[:, :], ALU.add, [pk, "brt"], [RT_])
                                red(rt[:, 36:37], rt[:, 0:4], ALU.max, [RT_], [RT_])
                                ts(rt[:, 48:49], rt[:, 36:37], -1.0, None, ALU.mult, None, [RT_], [RT_])
                                act(rt[:, 40:44], rt[:, 0:4], AF.Exp, [RT_], [RT_], bias=rt[:, 48:49], accum_out=rt[:, 37:38])
                                recip(rt[:, 37:38], rt[:, 37:38], [RT_], [RT_])
                                ts(rt[:, 40:44], rt[:, 0:4], rt[:, 36:37], None, ALU.is_equal, None, [RT_], [RT_])
                                ts(rt[:, 44:48], rt[:, 40:44], -1.0, BIG, ALU.add, ALU.mult, [RT_], [RT_])
                                for g in range(4):
                                    ts(rtb[:, 0, g * 8:(g + 1) * 8], rt[:, 4 + g * 8:12 + g * 8], rt[:, 44 + g:45 + g], None, ALU.add, None, [RT_], ["rtb"])
                                red(rt[:, 38:39], rtb[:, 0, :], ALU.max, ["rtb"], [RT_])
                                ts(O1a[:, ti, :], rtb[:, 0, :], rt[:, 38:39], None, ALU.is_equal, None, ["rtb", RT_], [("O1", ti)])
                                stt(rtb[:, 1, :], O1a[:, ti, :], -BIG, rtb[:, 0, :], ALU.mult, ALU.add, [("O1", ti), "rtb"], ["rtb"])
                                red(rt[:, 39:40], rtb[:, 1, :], ALU.max, ["rtb"], [RT_])
                                ts(O2a[:, ti, :], rtb[:, 1, :], rt[:, 39:40], None, ALU.is_equal, None, ["rtb", RT_], [("O2", ti)])
                                tt(rt[:, 48:49], rt[:, 39:40], rt[:, 38:39], ALU.subtract, [RT_], [RT_])
                                act(rt[:, 48:49], rt[:, 48:49], AF.Exp, [RT_], [RT_])
                                ts(rt[:, 48:49], rt[:, 48:49], 1.0, None, ALU.add, None, [RT_], [RT_])
                                recip(rt[:, 48:49], rt[:, 48:49], [RT_], [RT_])
                                tt(w12[:, ti, 0:1], rt[:, 48:49], rt[:, 37:38], ALU.mult, [RT_], [("w12", ti)])
                                tt(w12[:, ti, 1:2], rt[:, 37:38], w12[:, ti, 0:1], ALU.subtract, [RT_, ("w12", ti)], [("w12", ti)])
                                tt(rtb[:, 2, :], O1a[:, ti, :], O2a[:, ti, :], ALU.add, [("O1", ti), ("O2", ti)], ["rtb"])
                                ps, pk = rot_mm.next()
                                mm(ps[:, 0:NE], trif[:], rtb[:, 2, :], True, True, ["c_tri", "rtb"], [pk])
                                tt(rtb[:, 3, :], ps[:, 0:NE], cum[:], ALU.add, [pk, "cum"], ["rtb"])
                                ps2, pk2 = rot_mm.next()
                                mm(ps2[:, 0:NE], onesf[:], rtb[:, 2, :], True, True, ["onesf", "rtb"], [pk2])
                                tt(cum[:], cum[:], ps2[:, 0:NE], ALU.add, ["cum", pk2], ["cum"])
                                tt(rtb[:, 0, :], O1a[:, ti, :], rtb[:, 3, :], ALU.mult, [("O1", ti), "rtb"], ["rtb"])
                                red(r12[:, ti, 0:1], rtb[:, 0, :], ALU.add, ["rtb"], [("r12", ti)])
                                tt(rtb[:, 1, :], O2a[:, ti, :], rtb[:, 3, :], ALU.mult, [("O2", ti), "rtb"], ["rtb"])
                                red(r12[:, ti, 1:2], rtb[:, 1, :], ALU.add, ["rtb"], [("r12", ti)])
                                if cfg.debug and l == 0:
                                    dma("sp", dbg["rt"][ti], rt[:, :], reads=["rt"])

                            gens = [tile_gen(tl) for tl in range(ntl)]
                            next(gens[0])
                            for tl in range(ntl):
                                if tl + 1 < ntl:
                                    next(gens[tl + 1])
                                for _ in gens[tl]:
                                    pass

                stop_at("pass2")
                nmt = tile_ctr[0]
                ts(rtb[:, 0, :], cum[:], float(CH - 1), None, ALU.add, None, ["cum"], ["rtb"])
                cp(pad_i[:], rtb[:, 0, :], ["rtb"], ["pad_i"])
                sh = int(np.log2(CH))
                ts(pad_i[:], pad_i[:], sh, None, ALU.arith_shift_right, None, ["pad_i"], ["pad_i"])
                ts(pad_i[:], pad_i[:], sh, None, ALU.logical_shift_left, None, ["pad_i"], ["pad_i"])
                cp(rtb[:, 1, :], pad_i[:], ["pad_i"], ["rtb"])
                memset(pst[:, 0:1], 0.0, ["pst"])
                for e in range(NE):
                    tt(pst[:, e + 1:e + 2], pst[:, e:e + 1], rtb[:, 1, e:e + 1], ALU.add, ["pst", "rtb"], ["pst"])
                memset(cexp[:], 0.0, ["cexp"])
                for e in range(NE):
                    ts(ctmp[:], jpos[:], pst[:, e + 1:e + 2], None, ALU.is_ge, None, ["c_jpos", "pst"], ["ctmp"])
                    tt(cexp[:], cexp[:], ctmp[:], ALU.add, ["cexp", "ctmp"], ["cexp"])
                ts(cexp[:], cexp[:], float(NE - 1), float(l * NE), ALU.min, ALU.add, ["cexp"], ["cexp"])
                ts(ctmp[:], cexp[:], 128.0, pidx[:, 0:1], ALU.mult, ALU.add, ["cexp", "c_pidx"], ["ctmp"])
                cp(gidx[:], ctmp[:], ["ctmp"], ["gidx"])
                for ti in range(nmt):
                    for k, Oa, Ok in ((0, O1a, "O1"), (1, O2a, "O2")):
                        tt(rtb[:, k, :], Oa[:, ti, :], pst[:, 0:NE], ALU.mult, [(Ok, ti), "pst"], ["rtb"])
                        red(rt[:, k:k + 1], rtb[:, k, :], ALU.add, ["rtb"], ["rt"])
                    tt(rt[:, 2:4], rt[:, 0:2], r12[:, ti, :], ALU.add, ["rt", ("r12", ti)], ["rt"])
                    cp(dst[:, ti, :], rt[:, 2:4], ["rt"], [("dst", ti)])
                if cfg.debug and l == 0:
                    dbr = sb("dbr", [128, NT, 6], F32)
                    memset(dbr[:], 0.0, ["dbr"])
                    for ti in range(nmt):
                        cp(dbr[:, ti, 0:2], dst[:, ti, :], [("dst", ti)], ["dbr"])
                        cp(dbr[:, ti, 2:4], w12[:, ti, :], [("w12", ti)], ["dbr"])
                        cp(dbr[:, ti, 4:6], r12[:, ti, :], [("r12", ti)], ["dbr"])
                    dma("sp", dbg["route"], dbr[:], reads=["dbr"])
                stop_at("disp")
                xbw_keys = [("xbw", n) for n in range(2 * nmt)]
                for ti, (r0, mj) in enumerate(moe_tiles):
                    xbt, xbk = xb[ti % 2], "xb%d" % (ti % 2)
                    dma("sp", xbt[:], h2buf[r0:r0 + 128, :], reads=[("h2buf", r0)], writes=[xbk])
                    for k in range(2):
                        op("pool", lambda e, xbt=xbt, ti=ti, k=k: e.indirect_dma_start(
                            out=xbuf[:, :], out_offset=bass.IndirectOffsetOnAxis(ap=dst[:, ti, k:k + 1], axis=0), in_=xbt[:, :], in_offset=None),
                            reads=[xbk, ("dst", ti)], writes=[("xbw", 2 * ti + k)], dma=True)

                stop_at("scatter")
                nch_used = -(-(2 * nmt * 128) // CH) + NE
                subs = [(j, m) for j in range(nch_used) for m in range(MCH)]

                def stage_a(si):
                    j, m = subs[si]
                    wi = j % 2
                    if m == 0:
                        for (wt_, wname, src) in ((wg[wi], wgk[wi], w_gate), (wu[wi], wuk[wi], w_up), (wd[wi], wdk[wi], w_down)):
                            op("pool", lambda e, wt_=wt_, src=src, j=j: e.indirect_dma_start(
                                out=wt_, out_offset=None, in_=src[:, :], in_offset=bass.IndirectOffsetOnAxis(ap=gidx[:, j:j + 1], axis=0)),
                                reads=["gidx"], writes=[wname], dma=True)
                    s0 = j * CH + m * 128
                    xi = si % 2
                    xbt, xbk = xb[xi], "xb%d" % xi
                    xbT, xbTk = xbT2[xi], "xbT%d" % xi
                    xv = xbt[:].rearrange("s (p c) -> s c p", c=8)
                    for hf in range(2):
                        ps, pk = rot_tr.next()
                        psb = ps[:].bitcast(BF16)
                        for c4 in range(4):
                            c = hf * 4 + c4
                            tr(psb[:, c4 * 128:(c4 + 1) * 128], xv[:, c, :], identb[:], [xbk, "cb_ident"], [pk], signal=(c4 == 3))
                        act(xbT[:, hf * 4:hf * 4 + 4, :], psb[:, 0:512].rearrange("p (a b) -> p a b", b=128), AF.Identity, [pk], [xbTk])

                def stage_b(si):
                    j, m = subs[si]
                    wi = j % 2
                    xi = si % 2
                    xbT, xbTk = xbT2[xi], "xbT%d" % xi
                    psg, pgk = rot_acc.next()
                    for c in range(8):
                        mm(psg[:, :], xbT[:, c, :], wg[wi][:, c * DE:(c + 1) * DE], c == 0, c == 7, [xbTk, wgk[wi]], [pgk], signal=(c == 7))
                    psu, puk = rot_mm.next()
                    for c in range(8):
                        mm(psu[:, :], xbT[:, c, :], wu[wi][:, c * DE:(c + 1) * DE], c == 0, c == 7, [xbTk, wuk[wi]], [puk], signal=(c == 7))
                    return (psg, pgk, psu, puk)

                def stage_c(si, st):
                    j, m = subs[si]
                    wi = j % 2
                    psg, pgk, psu, puk = st
                    s0 = j * CH + m * 128
                    xi = si % 2
                    a_sb, a_sbk = a_sb2[xi], "a_sb%d" % xi
                    aT, aTk = aT2[xi], "aT%d" % xi
                    ob, obk = ob2[xi], ob2k[xi]
                    tsl, tslk = t512[xi], "t512_%d" % xi
                    act(tsl[:, :], psg[:, :], AF.Silu, [pgk], [tslk])
                    tt(a_sb[:, :], tsl[:, :], psu[:, :], ALU.mult, [tslk, puk], [a_sbk])
                    av = a_sb[:].rearrange("s (p c) -> s c p", c=4)
                    ps, pk = rot_tr.next()
                    psb = ps[:].bitcast(BF16)
                    for c in range(4):
                        tr(psb[:, c * 128:(c + 1) * 128], av[:, c, :], identb[:], [a_sbk, "cb_ident"], [pk], signal=(c == 3))
                    cp(aT[:], psb[:, 0:512].rearrange("p (a b) -> p a b", b=128), [pk], [aTk])
                    for half in range(2):
                        ps, pk = rot_d.next()
                        for c in range(4):
                            mm(ps[:, :], aT[:, c, :], wd[wi][:, c * D + half * 512:c * D + half * 512 + 512], c == 0, c == 3, [aTk, wdk[wi]], [pk], signal=(c == 3))
                        if half == 0:
                            act(ob[:, 0:512], ps[:, :], AF.Identity, [pk], [obk])
                        else:
                            cp(ob[:, 512:1024], ps[:, :], [pk], [obk])
                    dma("sp", obuf[s0:s0 + 128, :], ob[:], reads=[obk], writes=["obuf"])

                def load_sub(si):
                    j, m = subs[si]
                    s0 = j * CH + m * 128
                    dma("sp", xb[si % 2][:], xbuf[s0:s0 + 128, :], reads=xbw_keys, writes=["xb%d" % (si % 2)])

                assert MCH >= 3
                nsub = len(subs)
                load_sub(0)
                load_sub(1)
                stage_a(0)
                load_sub(2)
                stage_a(1)
                st_next = stage_b(0)
                for si in range(nsub):
                    st_cur = st_next
                    if si + 2 < nsub:
                        stage_a(si + 2)
                    if si + 3 < nsub:
                        load_sub(si + 3)
                    if si + 1 < nsub:
                        st_next = stage_b(si + 1)
                    stage_c(si, st_cur)

                stop_at("experts")
                if last:
                    dma("sp", sh2bc[:], final_g.partition_broadcast(128), writes=["sh2bc"])
                cur_mj = None
                for ti, (r0, mj) in enumerate(moe_tiles):
                    if mj != cur_mj:
                        build_bc(g1bc, "g1bc", modT, "modT", 40, mj)
                        cur_mj = mj
                    o12c, o12ck = o12s[ti % 2], o12sk[ti % 2]
                    for k in range(2):
                        op("pool", lambda e, ti=ti, k=k, o12c=o12c: e.indirect_dma_start(
                            out=o12c[k], out_offset=None, in_=obuf[:, :], in_offset=bass.IndirectOffsetOnAxis(ap=dst[:, ti, k:k + 1], axis=0)),
                            reads=["obuf", ("dst", ti)], writes=[o12ck[k]], dma=True)
                    if ti == 0:
                        cpre = load_x(1, r0)
                    xtile, xk = cpre
                    if ti + 1 < len(moe_tiles):
                        cpre = load_x(1, moe_tiles[ti + 1][0])
                    act(o12c[0], o12c[0], AF.Identity, [o12ck[0], ("w12", ti)], [o12ck[0]], scale=w12[:, ti, 0:1])
                    stt(o12c[0], o12c[1], w12[:, ti, 1:2], o12c[0], ALU.mult, ALU.add, [o12ck[1], o12ck[0], ("w12", ti)], [o12ck[0]])
                    tt(o12c[0], o12c[0], g1bc[:], ALU.mult, [o12ck[0], "g1bc"], [o12ck[0]])
                    tt(xtile[:], xtile[:], o12c[0], ALU.add, [xk, o12ck[0]], [xk])
                    if not last:
                        dma("sp", xres[r0:r0 + 128, :], xtile[:], reads=[xk], writes=[("xres", r0)])
                    else:
                        rstd_of(xtile, xk, 0, h2f[:], ["h2f"])
                        stt(xtile[:], xtile[:], ss[:, 0:1], sh2bc[:], ALU.mult, ALU.mult, [xk, ("ss", 0), "sh2bc"], [xk])
                        dma("sp", out[r0:r0 + 128, :], xtile[:], reads=[xk])
        except StopBuild:
            pass
        Sx.emit()
        build.sb_bytes = Sx.sb_bytes
        build.counts = {e: len(s) for e, s in Sx.streams.items()}
    return nc


def _perm_in():
    aq, ak, av, bu, cq, ck, cv = 0, 384, 512, 640, 1152, 1536, 1664
    cols = []
    for i in range(3):
        cols += list(range(aq + i * 64, aq + i * 64 + 64)) + list(range(aq + (i + 3) * 64, aq + (i + 3) * 64 + 64))
    for i in range(3):
        cols += list(range(cq + i * 64, cq + i * 64 + 64)) + list(range(cq + (i + 3) * 64, cq + (i + 3) * 64 + 64))
    cols += list(range(ak, ak + 128))
    cols += list(range(bu, bu + 512))
    cols += list(range(ck, ck + 128))
    cols += list(range(av, av + 128)) + list(range(cv, cv + 128))
    return np.array(cols)


def _perm_out():
    rows = []
    for i in range(3):
        rows += list(range(i * 64, i * 64 + 64)) + list(range((i + 3) * 64, (i + 3) * 64 + 64))
    rows += list(range(384, 640))
    for i in range(3):
        rows += list(range(640 + i * 64, 640 + i * 64 + 64)) + list(range(640 + (i + 3) * 64, 640 + (i + 3) * 64 + 64))
    return np.array(rows)


def make_in_maps(cfg, n_cores, x, c, ctx, c_ctx, norm1_g, norm2_g, w_mod, b_mod, w_in, q_norm_g, k_norm_g,
                 conv_w, conv_b, conv_ln_g, conv_ln_b, sink, w_out, w_group, b_group,
                 w_expert, b_expert, w_gate, w_up, w_down, final_g):
    f = lambda a: np.ascontiguousarray(np.asarray(a, dtype=np.float32))
    NB, S, DEPTH = cfg.NB, cfg.S, cfg.DEPTH
    x, c, ctx, c_ctx = f(x), f(c), f(ctx), f(c_ctx)
    shared = {}
    shared["n1gT"] = f(f(norm1_g).reshape(DEPTH, 8, 128).transpose(0, 2, 1))
    shared["n2gT"] = f(f(norm2_g).reshape(DEPTH, 8, 128).transpose(0, 2, 1))
    shared["w_mod"] = f(w_mod)
    shared["bmodT"] = f(f(b_mod).reshape(DEPTH, 48, 128).transpose(0, 2, 1))
    shared["b_mod"] = f(b_mod)
    shared["w_in"] = f(f(w_in)[:, :, _perm_in()])
    gq = np.tile(f(q_norm_g), (1, 2))
    gk = np.tile(f(k_norm_g), (1, 2))
    shared["gqk"] = f(np.stack([gq, gk], axis=-1))
    cw = f(conv_w)[:, :, 0, :]
    shared["convwT"] = f(cw.reshape(DEPTH, CONV_W, 2, 128).transpose(0, 3, 2, 1))
    cpar = np.stack([f(conv_b), f(conv_ln_g), f(conv_ln_b)], axis=-1)
    shared["convp"] = f(cpar.reshape(DEPTH, 2, 128, 3).transpose(0, 2, 1, 3))
    sk = f(sink)
    sb_ = np.zeros((DEPTH, 128, 3), np.float32)
    sb_[:, :64, :] = sk[:, None, 0:3]
    sb_[:, 64:, :] = sk[:, None, 3:6]
    shared["sinkb"] = sb_
    shared["w_out"] = f(f(w_out)[:, _perm_out(), :])
    shared["wr"] = f(np.concatenate([f(w_group), f(w_expert)], axis=-1))
    shared["br"] = f(np.concatenate([f(b_group), f(b_expert)], axis=-1))
    shared["w_gate"] = f(w_gate).reshape(DEPTH * NE * 128, 8 * DE)
    shared["w_up"] = f(w_up).reshape(DEPTH * NE * 128, 8 * DE)
    shared["w_down"] = f(w_down).reshape(DEPTH * NE * 128, 4 * D)
    shared["final_g"] = f(final_g)
    for k, v in host_consts(cfg).items():
        shared["k_" + k] = v
    maps = []
    for ci in range(n_cores):
        bs = slice(ci * NB, (ci + 1) * NB)
        m = dict(shared)
        m["x_in"] = f(x[bs].reshape(NB * S, D))
        m["ctx_in"] = f(ctx[bs].reshape(NB * L, D))
        cc = np.concatenate([c[bs], c_ctx[None, :]], axis=0)
        m["cT"] = f(cc.reshape(NB + 1, 8, 128).transpose(2, 1, 0))
        maps.append(m)
    return maps


_NC_CACHE = {}


def kernel(**inputs):
    x = np.asarray(inputs["x"])
    B, S, _ = x.shape
    DEPTH = np.asarray(inputs["w_in"]).shape[0]
    n_cores = 8
    NB = B // n_cores
    cfg = Cfg(NB=NB, S=S, DEPTH=DEPTH)
    key = (NB, S, DEPTH)
    if key not in _NC_CACHE:
        _NC_CACHE[key] = build(cfg)
    nc = _NC_CACHE[key]
    maps = make_in_maps(cfg, n_cores, **inputs)
    res = run_bass_kernel_spmd(nc, maps, core_ids=list(range(n_cores)))
    outs = [np.asarray(r["out"]).reshape(NB, S, D) for r in res.results]
    return np.concatenate(outs, axis=0).astype(np.float32)
```
